# Optimizing a Trainium2 kernel written in Bass

```python
import math
import jax
import jax.numpy as jnp
from jax import lax
import numpy as np

D_MODEL = 1024
BATCH = 2
SEQ = 16384
DEPTH = 4

CTX_LEN = 256
GRID_W = 64
N_MIXERS = 3
EPS = 1e-6
NEG_INF = -1e30
ROPE_BASE = 10000.0

ML_HEADS = 8
ML_V = D_MODEL // ML_HEADS
ML_QK = ML_V // 2
ML_CHUNK = 128
ML_QKW = ML_HEADS * ML_QK
ML_IN = 2 * ML_QKW + 2 * D_MODEL + 4 * ML_HEADS
ML_SPLITS = (ML_QKW, 2 * ML_QKW, 2 * ML_QKW + D_MODEL, 2 * ML_QKW + 2 * D_MODEL)

DF_HD = 64
DF_HEADS = D_MODEL // (2 * DF_HD)
DF_VD = 2 * DF_HD
DF_QKW = 2 * DF_HEADS * DF_HD
DF_IN = 2 * DF_QKW + DF_HEADS * DF_VD

SW_HD = 64
SW_HEADS = D_MODEL // SW_HD
SW_KV = 4
SW_GROUP = SW_HEADS // SW_KV
SW_WIN = 128
SW_BLOCK = 128
SW_IN = SW_HEADS * SW_HD + 2 * SW_KV * SW_HD

Q_BLOCK = 128

FF_DENSE = 11 * D_MODEL // 4
N_EXPERTS = 8
TOP_K = 2
FF_EXPERT = 7 * D_MODEL // 2

N_A = (DEPTH + 2) // 3
N_B = (DEPTH + 1) // 3
N_C = DEPTH // 3
N_DENSE = (DEPTH + 1) // 2
N_MOE = DEPTH // 2

kernel_name = "hybrid_mlstm_diffattn_swa_moe_dit"


def _rmsnorm(x, g):
    xf = x.astype(jnp.float32)
    y = xf * lax.rsqrt(jnp.mean(xf * xf, axis=-1, keepdims=True) + EPS)
    return (y * g.astype(jnp.float32)).astype(x.dtype)


def _modulate(h, shift, scale):
    return h * (1 + scale) + shift


def _rope_2d(L, hd):
    rows = L // GRID_W
    row = jnp.repeat(jnp.arange(rows, dtype=jnp.float32), GRID_W)
    col = jnp.tile(jnp.arange(GRID_W, dtype=jnp.float32), rows)
    nf = hd // 4
    inv = jnp.power(ROPE_BASE, -jnp.arange(nf, dtype=jnp.float32) / nf)
    ang = jnp.concatenate([row[:, None] * inv, col[:, None] * inv], axis=-1)
    return jnp.cos(ang), jnp.sin(ang)


def _apply_rope(x, cos, sin):
    x1, x2 = jnp.split(x.astype(jnp.float32), 2, axis=-1)
    c, s = cos[:, None, :], sin[:, None, :]
    return jnp.concatenate([x1 * c - x2 * s, x1 * s + x2 * c], axis=-1).astype(x.dtype)


def _mlstm_scan(q, k, v, i_pre, f_pre, state, with_out):
    B, n, H, dqk = q.shape
    dv = v.shape[-1]
    nc = n // ML_CHUNK
    qc = q.reshape(B, nc, ML_CHUNK, H, dqk)
    kc = k.reshape(B, nc, ML_CHUNK, H, dqk)
    vc = v.reshape(B, nc, ML_CHUNK, H, dv)
    ig = i_pre.astype(jnp.float32).reshape(B, nc, ML_CHUNK, H)
    b = jnp.cumsum(jax.nn.log_sigmoid(f_pre.astype(jnp.float32)).reshape(B, nc, ML_CHUNK, H), axis=2)
    b_end = b[:, :, -1]
    w_end = b_end[:, :, None] + ig - b
    m_loc = jnp.max(w_end, axis=2)
    e_end = jnp.exp(w_end - m_loc[:, :, None])
    C_loc = jnp.einsum('bcshv,bcshd->bchvd', e_end[..., None] * vc, kc)
    n_loc = jnp.einsum('bcsh,bcshd->bchd', e_end, kc)

    def step(carry, xs):
        C, nv, m = carry
        Cl, nl, ml, bl = xs
        m_new = jnp.maximum(bl + m, ml)
        a = jnp.exp(bl + m - m_new)
        s = jnp.exp(ml - m_new)
        C_new = a[..., None, None] * C + s[..., None, None] * Cl
        n_new = a[..., None] * nv + s[..., None] * nl
        return (C_new, n_new, m_new), (C, nv, m)

    xs = (jnp.moveaxis(C_loc, 1, 0), jnp.moveaxis(n_loc, 1, 0),
          jnp.moveaxis(m_loc, 1, 0), jnp.moveaxis(b_end, 1, 0))
    final, (C_in, n_in, m_in) = lax.scan(step, state, xs)
    if not with_out:
        return None, final
    C_in = jnp.moveaxis(C_in, 0, 1)
    n_in = jnp.moveaxis(n_in, 0, 1)
    m_in = jnp.moveaxis(m_in, 0, 1)
    t_idx = jnp.arange(ML_CHUNK)
    lower = t_idx[:, None] >= t_idx[None, :]
    d_log = b[:, :, :, None, :] - b[:, :, None, :, :] + ig[:, :, None, :, :]
    d_log = jnp.where(lower[None, None, :, :, None], d_log, -jnp.inf)
    m_inter = b + m_in[:, :, None, :]
    m_t = jnp.maximum(m_inter, jnp.max(d_log, axis=3))
    w = jnp.exp(d_log - m_t[:, :, :, None, :]) * jnp.einsum('bcthd,bcshd->bctsh', qc, kc)
    sc = jnp.exp(m_inter - m_t)
    num = jnp.einsum('bctsh,bcshv->bcthv', w, vc) + sc[..., None] * jnp.einsum('bchvd,bcthd->bcthv', C_in, qc)
    den = jnp.sum(w, axis=3) + sc * jnp.einsum('bchd,bcthd->bcth', n_in, qc)
    h = num / jnp.maximum(jnp.abs(den), jnp.exp(-m_t))[..., None]
    return h.reshape(B, n, H, dv).astype(v.dtype), final


def _mlstm_mixer(h_lat, h_ctx, w_in, gate_b, hnorm_g, w_out, need_ctx_out):
    B = h_lat.shape[0]

    def proj(h):
        n = h.shape[1]
        q, k, v, o, g = jnp.split(h @ w_in, ML_SPLITS, axis=-1)
        q = q.reshape(B, n, ML_HEADS, ML_QK)
        k = k.reshape(B, n, ML_HEADS, ML_QK) * (ML_QK ** -0.5)
        v = v.reshape(B, n, ML_HEADS, ML_V)
        g = g.reshape(B, n, 4, ML_HEADS).astype(jnp.float32) + gate_b.astype(jnp.float32)
        return q, k, v, o, g

    flip = lambda a: a[:, ::-1]

    def bidir(h, st_f, st_b, with_out):
        q, k, v, o, g = proj(h)
        hf, sf = _mlstm_scan(q, k, v, g[:, :, 0], g[:, :, 1], st_f, with_out)
        hb, sb = _mlstm_scan(flip(q), flip(k), flip(v), flip(g[:, :, 2]), flip(g[:, :, 3]), st_b, with_out)
        if not with_out:
            return None, sf, sb
        y = _rmsnorm(hf + flip(hb), hnorm_g.reshape(ML_HEADS, ML_V)).reshape(B, h.shape[1], D_MODEL)
        return (y * jax.nn.sigmoid(o)) @ w_out, sf, sb

    zero = (jnp.zeros((B, ML_HEADS, ML_V, ML_QK), jnp.float32),
            jnp.zeros((B, ML_HEADS, ML_QK), jnp.float32),
            jnp.zeros((B, ML_HEADS), jnp.float32))
    y_c, sf, sb = bidir(h_ctx, zero, zero, need_ctx_out)
    y_l, _, _ = bidir(h_lat, sf, sb, True)
    return y_l, y_c


def _diff_attend(q, k, v, lam):
    s = jnp.einsum('bqhd,bkhd->bhqk', q, k).astype(jnp.float32) * (DF_HD ** -0.5)
    p = jax.nn.softmax(s, axis=-1)
    B, H2, nq, nk = p.shape
    p = p.reshape(B, H2 // 2, 2, nq, nk)
    a = p[:, :, 0] - lam * p[:, :, 1]
    return jnp.einsum('bhqk,bkhd->bqhd', a.astype(v.dtype), v)


def _diff_mixer(h_lat, h_ctx, w_in, lam, hnorm_g, w_out, lambda_init, cos, sin, need_ctx_out):
    B, L, _ = h_lat.shape
    lam = lam.astype(jnp.float32)
    lam_full = jnp.exp(jnp.sum(lam[0] * lam[1])) - jnp.exp(jnp.sum(lam[2] * lam[3])) + lambda_init

    def proj(h):
        n = h.shape[1]
        q, k, v = jnp.split(h @ w_in, (DF_QKW, 2 * DF_QKW), axis=-1)
        return (q.reshape(B, n, 2 * DF_HEADS, DF_HD), k.reshape(B, n, 2 * DF_HEADS, DF_HD),
                v.reshape(B, n, DF_HEADS, DF_VD))

    def finish(o):
        n = o.shape[1]
        o = _rmsnorm(o, hnorm_g.reshape(DF_HEADS, DF_VD)) * (1 - lambda_init)
        return o.reshape(B, n, D_MODEL) @ w_out

    qc, kc, vc = proj(h_ctx)
    ql, kl, vl = proj(h_lat)
    ql, kl = _apply_rope(ql, cos, sin), _apply_rope(kl, cos, sin)
    k_all = jnp.concatenate([kc, kl], axis=1)
    v_all = jnp.concatenate([vc, vl], axis=1)
    nb = L // Q_BLOCK
    q_blocks = jnp.moveaxis(ql.reshape(B, nb, Q_BLOCK, 2 * DF_HEADS, DF_HD), 1, 0)
    o_l = lax.map(lambda qb: _diff_attend(qb, k_all, v_all, lam_full), q_blocks)
    y_l = finish(jnp.moveaxis(o_l, 0, 1).reshape(B, L, DF_HEADS, DF_VD))
    y_c = finish(_diff_attend(qc, kc, vc, lam_full)) if need_ctx_out else None
    return y_l, y_c


def _sink_attend(q, k, v, sinks, mask):
    s = jnp.einsum('bqkgd,bnkd->bkgqn', q, k).astype(jnp.float32) * (SW_HD ** -0.5)
    if mask is not None:
        s = jnp.where(mask, s, NEG_INF)
    sink = jnp.broadcast_to(sinks.astype(jnp.float32)[None, :, :, None, None], s.shape[:-1] + (1,))
    p = jax.nn.softmax(jnp.concatenate([sink, s], axis=-1), axis=-1)[..., 1:]
    return jnp.einsum('bkgqn,bnkd->bqkgd', p.astype(v.dtype), v)


def _swa_mixer(h_lat, h_ctx, w_in, sinks, w_out, cos, sin, need_ctx_out):
    B, L, _ = h_lat.shape
    sinks = sinks.reshape(SW_KV, SW_GROUP)

    def proj(h):
        n = h.shape[1]
        q, k, v = jnp.split(h @ w_in, (SW_HEADS * SW_HD, SW_HEADS * SW_HD + SW_KV * SW_HD), axis=-1)
        return (q.reshape(B, n, SW_HEADS, SW_HD), k.reshape(B, n, SW_KV, SW_HD), v.reshape(B, n, SW_KV, SW_HD))

    qc, kc, vc = proj(h_ctx)
    ql, kl, vl = proj(h_lat)
    ql, kl = _apply_rope(ql, cos, sin), _apply_rope(kl, cos, sin)
    nb = L // SW_BLOCK
    pad = ((0, 0), (SW_BLOCK, SW_BLOCK), (0, 0), (0, 0))
    kp = jnp.pad(kl, pad).reshape(B, nb + 2, SW_BLOCK, SW_KV, SW_HD)
    vp = jnp.pad(vl, pad).reshape(B, nb + 2, SW_BLOCK, SW_KV, SW_HD)
    k_band = jnp.concatenate([kp[:, :-2], kp[:, 1:-1], kp[:, 2:]], axis=2)
    v_band = jnp.concatenate([vp[:, :-2], vp[:, 1:-1], vp[:, 2:]], axis=2)
    blk = jnp.arange(nb)[:, None, None] * SW_BLOCK
    qpos = blk + jnp.arange(SW_BLOCK)[None, :, None]
    kpos = blk - SW_BLOCK + jnp.arange(3 * SW_BLOCK)[None, None, :]
    band_mask = (jnp.abs(qpos - kpos) <= SW_WIN) & (kpos >= 0) & (kpos < L)
    ctx_mask = jnp.ones((SW_BLOCK, kc.shape[1]), dtype=bool)
    q_blocks = jnp.moveaxis(ql.reshape(B, nb, SW_BLOCK, SW_KV, SW_GROUP, SW_HD), 1, 0)

    def one_block(xs):
        qb, kb, vb, mb = xs
        k = jnp.concatenate([kc, kb], axis=1)
        v = jnp.concatenate([vc, vb], axis=1)
        return _sink_attend(qb, k, v, sinks, jnp.concatenate([ctx_mask, mb], axis=1))

    o_l = lax.map(one_block, (q_blocks, jnp.moveaxis(k_band, 1, 0), jnp.moveaxis(v_band, 1, 0), band_mask))
    y_l = jnp.moveaxis(o_l, 0, 1).reshape(B, L, D_MODEL) @ w_out
    y_c = None
    if need_ctx_out:
        n_c = h_ctx.shape[1]
        o_c = _sink_attend(qc.reshape(B, n_c, SW_KV, SW_GROUP, SW_HD), kc, vc, sinks, None)
        y_c = o_c.reshape(B, n_c, D_MODEL) @ w_out
    return y_l, y_c


def _swiglu(h, w_in, w_out):
    a, b = jnp.split(h @ w_in, 2, axis=-1)
    return (jax.nn.silu(a) * b) @ w_out


def _moe(h, router, w_in, w_out):
    logits = (h @ router).astype(jnp.float32)
    top_v, top_i = lax.top_k(logits, TOP_K)
    p = jax.nn.softmax(top_v, axis=-1)
    gates = jnp.sum(jax.nn.one_hot(top_i, N_EXPERTS, dtype=jnp.float32) * p[..., None], axis=-2)
    y = jnp.zeros_like(h)
    for e in range(N_EXPERTS):
        y = y + gates[..., e:e + 1].astype(h.dtype) * _swiglu(h, w_in[e], w_out[e])
    return y


def setup_inputs(seed: int = 0) -> dict:
    key = jax.random.key(seed)
    ks = jax.random.split(key, 25)
    D = D_MODEL

    def nrm(k, shape, scale):
        return jax.random.normal(k, shape, jnp.float32) * scale

    forget_off = jnp.linspace(3.0, 6.0, ML_HEADS, dtype=jnp.float32)
    gate_off = jnp.stack([jnp.zeros_like(forget_off), forget_off, jnp.zeros_like(forget_off), forget_off])
    return {
        'x': nrm(ks[0], (BATCH, SEQ, D), 1.0),
        'c': nrm(ks[1], (BATCH, D), 1.0),
        'ctx': nrm(ks[2], (BATCH, CTX_LEN, D), 1.0),
        'c_ctx': nrm(ks[3], (D,), 1.0),
        'ada_w': nrm(ks[4], (DEPTH, D, 6 * D), 0.5 * D ** -0.5),
        'ada_b': nrm(ks[5], (DEPTH, 6 * D), 0.02),
        'norm1': 1.0 + nrm(ks[6], (DEPTH, D), 0.02),
        'norm2': 1.0 + nrm(ks[7], (DEPTH, D), 0.02),
        'ml_w_in': nrm(ks[8], (N_A, D, ML_IN), D ** -0.5),
        'ml_gate_b': gate_off + nrm(ks[9], (N_A, 4, ML_HEADS), 0.1),
        'ml_hnorm': 1.0 + nrm(ks[10], (N_A, D), 0.02),
        'ml_w_out': nrm(ks[11], (N_A, D, D), D ** -0.5),
        'df_w_in': nrm(ks[12], (N_B, D, DF_IN), D ** -0.5),
        'df_lam': nrm(ks[13], (N_B, 4, DF_HD), 0.1),
        'df_hnorm': 1.0 + nrm(ks[14], (N_B, D), 0.02),
        'df_w_out': nrm(ks[15], (N_B, D, D), D ** -0.5),
        'sw_w_in': nrm(ks[16], (N_C, D, SW_IN), D ** -0.5),
        'sw_sinks': nrm(ks[17], (N_C, SW_HEADS), 1.0),
        'sw_w_out': nrm(ks[18], (N_C, D, D), D ** -0.5),
        'ffn_w_in': nrm(ks[19], (N_DENSE, D, 2 * FF_DENSE), D ** -0.5),
        'ffn_w_out': nrm(ks[20], (N_DENSE, FF_DENSE, D), FF_DENSE ** -0.5),
        'moe_router': nrm(ks[21], (N_MOE, D, N_EXPERTS), D ** -0.5),
        'moe_w_in': nrm(ks[22], (N_MOE, N_EXPERTS, D, 2 * FF_EXPERT), D ** -0.5),
        'moe_w_out': nrm(ks[23], (N_MOE, N_EXPERTS, FF_EXPERT, D), FF_EXPERT ** -0.5),
        'final_norm': 1.0 + nrm(ks[24], (D,), 0.02),
    }


def reference(x, c, ctx, c_ctx, ada_w, ada_b, norm1, norm2, ml_w_in, ml_gate_b, ml_hnorm, ml_w_out,
              df_w_in, df_lam, df_hnorm, df_w_out, sw_w_in, sw_sinks, sw_w_out,
              ffn_w_in, ffn_w_out, moe_router, moe_w_in, moe_w_out, final_norm):
    L = x.shape[1]
    cos, sin = _rope_2d(L, DF_HD)
    cond_lat = jax.nn.silu(c)
    cond_ctx = jax.nn.silu(c_ctx)
    xc = ctx
    n_ctx = ctx.shape[1]
    for i in range(DEPTH):
        last = i == DEPTH - 1
        mod_l = [m[:, None] for m in jnp.split(cond_lat @ ada_w[i] + ada_b[i], 6, axis=-1)]
        mod_c = jnp.split(cond_ctx @ ada_w[i] + ada_b[i], 6, axis=-1)
        h_l = _modulate(_rmsnorm(x, norm1[i]), mod_l[0], mod_l[1])
        h_c = _modulate(_rmsnorm(xc, norm1[i]), mod_c[0], mod_c[1])
        kind, j = i % N_MIXERS, i // N_MIXERS
        if kind == 0:
            y_l, y_c = _mlstm_mixer(h_l, h_c, ml_w_in[j], ml_gate_b[j], ml_hnorm[j], ml_w_out[j], not last)
        elif kind == 1:
            lambda_init = 0.8 - 0.6 * math.exp(-0.3 * i)
            y_l, y_c = _diff_mixer(h_l, h_c, df_w_in[j], df_lam[j], df_hnorm[j], df_w_out[j],
                                   lambda_init, cos, sin, not last)
        else:
            y_l, y_c = _swa_mixer(h_l, h_c, sw_w_in[j], sw_sinks[j], sw_w_out[j], cos, sin, not last)
        x = x + mod_l[2] * y_l
        if not last:
            xc = xc + mod_c[2] * y_c
        h2 = _modulate(_rmsnorm(x, norm2[i]), mod_l[3], mod_l[4])
        if not last:
            h2_c = _modulate(_rmsnorm(xc, norm2[i]), mod_c[3], mod_c[4])
            h2 = jnp.concatenate([h2_c, h2], axis=1)
        jf = i // 2
        if i % 2 == 0:
            y2 = _swiglu(h2, ffn_w_in[jf], ffn_w_out[jf])
        else:
            y2 = _moe(h2, moe_router[jf], moe_w_in[jf], moe_w_out[jf])
        if last:
            x = x + mod_l[5] * y2
        else:
            xc = xc + mod_c[5] * y2[:, :n_ctx]
            x = x + mod_l[5] * y2[:, n_ctx:]
    return _rmsnorm(x, final_norm)
```

```python
import contextlib
import numpy as np
import concourse.bass as bass
import concourse.mybir as mybir

F32 = mybir.dt.float32
BF16 = mybir.dt.bfloat16
AF = mybir.ActivationFunctionType
ALU = mybir.AluOpType
AX = mybir.AxisListType

N_DMA_SEMS = 24
EPOCH_LIMIT = 28000


class Prog:
    ENGS = ("pe", "act", "dve", "pool", "sp")

    def __init__(self, nc, stack, n_epochs=6):
        self.nc = nc
        self.stack = stack
        self.eng_obj = {"pe": nc.tensor, "act": nc.scalar, "dve": nc.vector,
                        "pool": nc.gpsimd, "sp": nc.sync}
        self.lists = {e: [] for e in self.ENGS}
        self.n_epochs = n_epochs
        self.eng_sems = [{e: stack.enter_context(nc.semaphore(f"s_{e}_{k}")) for e in self.ENGS}
                         for k in range(n_epochs)]
        self.bar_sem = stack.enter_context(nc.semaphore("s_bar"))
        self.bar_count = 0
        self.dma_sems = [stack.enter_context(nc.semaphore(f"s_dma{i}")) for i in range(N_DMA_SEMS)]
        self.dma_vals = [0] * N_DMA_SEMS
        self.dma_next = 0
        self.epoch = 0
        self.cnt = {e: 0 for e in self.ENGS}
        self.state = {}
        self.waited = {e: {} for e in self.ENGS}
        self.n_ops = 0
        self.uid = 0
        self.scopes = []

    def sb(self, name, shape, dt):
        stk = self.scopes[-1] if self.scopes else self.stack
        return stk.enter_context(self.nc.sbuf_tensor(name, list(shape), dt))

    def ps(self, name, shape, dt):
        return self.stack.enter_context(self.nc.psum_tensor(name, list(shape), dt))

    def push_scope(self):
        self.scopes.append(contextlib.ExitStack())

    def pop_scope(self):
        self.barrier()
        self.scopes.pop().close()

    def _deps(self, reads, writes):
        deps = []
        for k in reads:
            st = self.state.get(k)
            if st and st[0] is not None:
                deps.append(st[0])
        for k in writes:
            st = self.state.get(k)
            if st:
                if st[0] is not None:
                    deps.append(st[0])
                deps.extend(st[1])
        return deps

    def _record(self, ev, reads, writes):
        for k in reads:
            st = self.state.setdefault(k, [None, []])
            st[1].append(ev)
        for k in writes:
            self.state[k] = [ev, []]

    def _emit_waits(self, eng, deps):
        w = self.waited[eng]
        best = {}
        for (sem, val) in deps:
            if w.get(id(sem), 0) >= val:
                continue
            if best.get(id(sem), (None, 0))[1] < val:
                best[id(sem)] = (sem, val)
        for sem, val in best.values():
            self.lists[eng].append(("wait", sem, val))
            w[id(sem)] = val

    def op(self, eng, fn, reads=(), writes=()):
        if self.cnt[eng] >= EPOCH_LIMIT:
            self.barrier()
        deps = self._deps(reads, writes)
        self._emit_waits(eng, deps)
        self.cnt[eng] += 1
        sem = self.eng_sems[self.epoch][eng]
        ev = (sem, self.cnt[eng])
        self.lists[eng].append(("op", fn, sem, 1))
        if eng == "pe":
            self.waited[eng][id(sem)] = self.cnt[eng]
        self._record(ev, reads, writes)
        self.n_ops += 1
        return ev

    def dma(self, queue, out, in_, reads=(), writes=(), **kw):
        i = self.dma_next
        self.dma_next = (self.dma_next + 1) % N_DMA_SEMS
        sem = self.dma_sems[i]
        deps = self._deps(reads, writes)
        if self.dma_vals[i] > 0:
            deps.append((sem, self.dma_vals[i]))
        self._emit_waits(queue, deps)
        self.dma_vals[i] += 16
        assert self.dma_vals[i] < 60000
        ev = (sem, self.dma_vals[i])

        def fn(e, out=out, in_=in_, kw=kw):
            return e.dma_start(out=out, in_=in_, **kw)
        self.lists[queue].append(("op", fn, sem, 16))
        self._record(ev, reads, writes)
        self.n_ops += 1
        return ev

    def barrier(self):
        live = []
        for st in self.state.values():
            if st[0] is not None:
                live.append(st[0])
            live.extend(st[1])
        dma_ids = {id(s) for s in self.dma_sems}
        live_dma = [ev for ev in live if id(ev[0]) in dma_ids]
        self._emit_waits("sp", live_dma)
        self.bar_count += 1
        n = len(self.ENGS)
        for e in self.ENGS:
            if self.cnt[e] > 0:
                self._emit_waits(e, [(self.eng_sems[self.epoch][e], self.cnt[e])])
            self.lists[e].append(("inc", self.bar_sem))
        for e in self.ENGS:
            self.lists[e].append(("wait", self.bar_sem, n * self.bar_count))
        self.epoch += 1
        assert self.epoch < self.n_epochs, "out of epochs"
        self.cnt = {e: 0 for e in self.ENGS}
        self.state = {}
        self.waited = {e: {} for e in self.ENGS}

    def finish(self, final_events):
        self._emit_waits("sp", list(final_events))

    def emit(self):
        nc = self.nc
        with nc.Block() as block:
            def mk(eng):
                def body(e):
                    for item in self.lists[eng]:
                        if item[0] == "wait":
                            e.wait_ge(item[1], item[2])
                        elif item[0] == "inc":
                            e.sem_inc(item[1], 1)
                        else:
                            ins = item[1](e)
                            ins.then_inc(item[2], item[3])
                return body
            block.tensor(mk("pe"))
            block.scalar(mk("act"))
            block.vector(mk("dve"))
            block.gpsimd(mk("pool"))
            block.sync(mk("sp"))

import math
from concourse.bass_utils import run_bass_kernel_spmd

D = 1024
SEQ = 16384
CTX = 256
NCORE = 8
I32 = mybir.dt.int32


class Rot:
    def __init__(self, P, name, shape, dt, n, psum=False):
        self.tiles = [(P.ps if psum else P.sb)(f"{name}{i}", shape, dt) for i in range(n)]
        self.keys = [f"{name}{i}" for i in range(n)]
        self.i = 0

    def next(self):
        t, k = self.tiles[self.i], self.keys[self.i]
        self.i = (self.i + 1) % len(self.tiles)
        return t, k


def dram_in(nc, name, shape, dt=F32):
    return nc.dram_tensor(name, list(shape), dt, kind="ExternalInput").ap()


def dram_out(nc, name, shape, dt=F32):
    return nc.dram_tensor(name, list(shape), dt, kind="ExternalOutput").ap()


def make_ident(P, name, dt, n=128):
    f = P.sb(name + "_f", [128, 128], F32)
    P.op("pool", lambda e: e.memset(f[:], 0.0), writes=[name + "_f"])
    P.op("pool", lambda e: e.affine_select(out=f[:], in_=f[:], pattern=[[-1, 128]], compare_op=ALU.not_equal,
                                           fill=1.0, base=0, channel_multiplier=1),
         reads=[name + "_f"], writes=[name + "_f"])
    if dt == F32:
        return f, name + "_f"
    b = P.sb(name, [128, 128], dt)
    P.op("dve", lambda e: e.tensor_copy(b[:], f[:]), reads=[name + "_f"], writes=[name])
    return b, name


def make_tri(P, name, upper, dt=F32):
    f = P.sb(name, [128, 128], dt)
    P.op("pool", lambda e: e.memset(f[:], 1.0), writes=[name])
    if upper:
        P.op("pool", lambda e: e.affine_select(out=f[:], in_=f[:], pattern=[[1, 128]], compare_op=ALU.is_ge,
                                               fill=0.0, base=0, channel_multiplier=-1), reads=[name], writes=[name])
    else:
        P.op("pool", lambda e: e.affine_select(out=f[:], in_=f[:], pattern=[[-1, 128]], compare_op=ALU.is_ge,
                                               fill=0.0, base=0, channel_multiplier=1), reads=[name], writes=[name])
    return f, name


def view(t, off, shape):
    n = 1
    for d_ in shape[1:]:
        n *= d_
    ap = t[:, off:off + n]
    if len(shape) == 3:
        ap = ap.rearrange("p (a b) -> p a b", b=shape[2])
    elif len(shape) == 4:
        ap = ap.rearrange("p (a b c) -> p a b c", b=shape[2], c=shape[3])
    return ap


class VRot:
    def __init__(self, name, views):
        self.tiles = views
        self.keys = [f"{name}{i}" for i in range(len(views))]
        self.i = 0

    def next(self):
        t, k = self.tiles[self.i], self.keys[self.i]
        self.i = (self.i + 1) % len(self.tiles)
        return t, k


def emit_mod(P, cond, ada_w, ada_b, chunks, ps_rot, tag, res, scr):
    cp = P.sb(tag + "cp", [128, 2, 8], F32)
    cs = P.sb(tag + "cs", [128, 2, 8], F32)
    ones = P.sb(tag + "ones", [128, 128], F32)
    cB = view(scr, 4096, [128, 2, 8, 128])
    P.dma("sp", cp[:], cond.rearrange("c (k p) -> p c k", p=128), writes=[tag + "cp"], allow_slow_non_contiguous=True)
    P.op("act", lambda e: e.activation(out=cs[:], in_=cp[:], func=AF.Silu), reads=[tag + "cp"], writes=[tag + "cs"])
    P.op("dve", lambda e: e.memset(ones[:], 1.0), writes=[tag + "ones"])
    for ci in range(2):
        for k in range(8):
            P.op("dve", lambda e, ci=ci, k=k: e.tensor_scalar(cB[:, ci, k, :], ones[:], cs[:, ci, k:k + 1], None, ALU.mult),
                 reads=[tag + "ones", tag + "cs"], writes=[tag + "cB"])
    MW = 256
    wrot = VRot(tag + "aw", [view(scr, i * 2048, [128, 8, MW]) for i in range(2)])
    brot = VRot(tag + "ab", [view(scr, 6144 + i * MW, [128, MW]) for i in range(2)])
    for j in chunks:
        for n in range(1024 // MW):
            c0 = j * 1024 + n * MW
            wt, wk = wrot.next()
            bt, bk = brot.next()
            P.dma("sp", wt, ada_w[:, c0:c0 + MW].rearrange("(k p) n -> p k n", p=128), writes=[wk])
            P.dma("sp", bt, ada_b[0:1, c0:c0 + MW].partition_broadcast(128), writes=[bk])
            for ci in range(2):
                pt, pk = ps_rot.next()
                for k in range(8):
                    P.op("pe", lambda e, pt=pt, wt=wt, ci=ci, k=k: e.matmul(pt[:, 0:MW], cB[:, ci, k, :], wt[:, k, :],
                                                                          start=(k == 0), stop=(k == 7)),
                         reads=[tag + "cB", wk], writes=[pk])
                t, tk = res[(ci, j)]
                P.op("dve", lambda e, t=t, pt=pt, bt=bt, n=n: e.tensor_tensor(t[:, n * MW:(n + 1) * MW], pt[:, 0:MW], bt, ALU.add),
                     reads=[pk, bk], writes=[tk])
    return res


def fold_gain(P, norm_g, mods, j_scale, tag):
    gb = P.sb(tag + "gb", [128, 1024], F32)
    P.dma("sp", gb[:], norm_g[0:1, :].partition_broadcast(128), writes=[tag + "gb"])
    for ci in range(2):
        t, tk = mods[(ci, j_scale)]
        P.op("dve", lambda e, t=t: e.scalar_tensor_tensor(t[:], t[:], 1.0, gb[:], ALU.add, ALU.mult),
             reads=[tk, tag + "gb"], writes=[tk])


class NormCtx:
    def __init__(self, P, tag, ident_bf, ident_key):
        self.P = P
        self.tag = tag
        self.sq = Rot(P, tag + "sq", [128, 1024], F32, 1)
        self.ss = Rot(P, tag + "ss", [128, 1], F32, 2)
        self.rs = Rot(P, tag + "rs", [128, 1], F32, 2)
        self.tmp = Rot(P, tag + "tmp", [128, 1024], F32, 1)
        self.hb = Rot(P, tag + "hb", [128, 1024], BF16, 2)
        self.ident, self.ik = ident_bf, ident_key

    def norm_mod(self, x, xk, rows, G, Gk, S, Sk, out_f32=None):
        P = self.P
        sq, sqk = self.sq.next()
        ss, ssk = self.ss.next()
        rs, rsk = self.rs.next()
        P.op("act", lambda e: e.activation(out=sq[:rows], in_=x[:rows], func=AF.Square, accum_out=ss[:rows]),
             reads=[xk], writes=[sqk, ssk])
        P.op("dve", lambda e: e.tensor_scalar(rs[:rows], ss[:rows], 1.0 / D, 1e-6, ALU.mult, ALU.add), reads=[ssk], writes=[rsk])
        P.op("act", lambda e: e.activation(out=rs[:rows], in_=rs[:rows], func=AF.Sqrt), reads=[rsk], writes=[rsk])
        P.op("dve", lambda e: e.reciprocal(rs[:rows], rs[:rows]), reads=[rsk], writes=[rsk])
        tmp, tk = self.tmp.next()
        P.op("dve", lambda e: e.scalar_tensor_tensor(tmp[:rows], x[:rows], rs[:rows, 0:1], G[:rows], ALU.mult, ALU.mult),
             reads=[xk, rsk, Gk], writes=[tk])
        hb, hk = self.hb.next()
        if out_f32 is not None:
            of, ofk = out_f32
            P.op("pool", lambda e: e.tensor_tensor(of[:rows], tmp[:rows], S[:rows], ALU.add), reads=[tk, Sk], writes=[ofk])
            P.op("act", lambda e: e.copy(hb[:rows], of[:rows]), reads=[ofk], writes=[hk])
        else:
            P.op("pool", lambda e: e.tensor_tensor(hb[:rows], tmp[:rows], S[:rows], ALU.add), reads=[tk, Sk], writes=[hk])
        return hb, hk


def transpose_to(P, src, srck, rows, dst_fn, dstk, ps_rot, ident, ik, dt=BF16, evac="act"):
    pt, pk = ps_rot.next()
    pv = pt[:].bitcast(dt) if dt != F32 else pt[:]
    nper = 8 if dt != F32 else 4
    for half in range(8 // nper):
        if half > 0:
            pt, pk = ps_rot.next()
            pv = pt[:]
        for kk in range(nper):
            k = half * nper + kk
            P.op("pe", lambda e, pv=pv, kk=kk, k=k: e.transpose(pv[:, kk * 128:kk * 128 + rows], src[:rows, k * 128:(k + 1) * 128], ident[:rows, :rows]),
                 reads=[srck, ik], writes=[pk])
        for kk in range(nper):
            k = half * nper + kk
            if evac == "act":
                P.op("act", lambda e, pv=pv, kk=kk, k=k: e.copy(dst_fn(k), pv[:, kk * 128:kk * 128 + rows]), reads=[pk], writes=[dstk])
            else:
                P.op("dve", lambda e, pv=pv, kk=kk, k=k: e.tensor_copy(dst_fn(k), pv[:, kk * 128:kk * 128 + rows]), reads=[pk], writes=[dstk])


def token_tiles(n_lat, n_ctx):
    tiles = []
    r = 0
    while r < n_lat:
        tiles.append((r, 128, 0))
        r += 128
    r = 0
    while r < n_ctx:
        rows = min(128, n_ctx - r)
        tiles.append((n_lat + r, rows, 1))
        r += rows
    return tiles


def load_w_bf16(P, dst, dstk, w, ncols, col0=0, kchunks=8, queue="pool"):
    for k in range(kchunks):
        P.dma(queue, dst[:, k, 0:ncols], w[k * 128:(k + 1) * 128, col0:col0 + ncols], writes=[dstk])


def build_A(n_lat, n_ctx, N, kind):
    nc = bass.Bass("TRN2", target_bir_lowering=False)
    NT = n_lat + n_ctx
    x = dram_in(nc, "x", [NT, D])
    cond = dram_in(nc, "cond", [2, D])
    ada_w = dram_in(nc, "ada_w", [D, 6 * D])
    ada_b = dram_in(nc, "ada_b", [1, 6 * D])
    ng = dram_in(nc, "norm_g", [1, D])
    w = dram_in(nc, "w_in", [D, N])
    rope = kind in ("df", "sw")
    if rope:
        pos = dram_in(nc, "pos", [n_lat])
    out = dram_out(nc, "proj", [NT, N])
    nh_rope = {"df": 32, "sw": 20}.get(kind, 0)
    with contextlib.ExitStack() as st:
        P = Prog(nc, st, n_epochs=4)
        psr = Rot(P, "ps", [128, 512], F32, 6, psum=True)
        ident, ik = make_ident(P, "ident", BF16)
        mod_tiles = {(ci, j): (P.sb(f"mmod{ci}_{j}", [128, 1024], F32), f"mmod{ci}_{j}") for j in [0, 1] for ci in range(2)}
        scr = P.sb("scr", [128, max(2 * N, 6656)], F32)
        mods = emit_mod(P, cond, ada_w, ada_b, [0, 1], psr, "m", mod_tiles, scr)
        fold_gain(P, ng, mods, 1, "m")
        P.barrier()
        W = P.sb("W", [128, 8, N], BF16)
        load_w_bf16(P, W, "W", w, N)
        ntl = n_lat // 128
        if rope:
            post = P.sb("post", [128, ntl], F32)
            P.dma("sp", post[:], pos.rearrange("(t p) -> p t", p=128), writes=["post"], allow_slow_non_contiguous=True)
            colt = P.sb("colt", [128, ntl], F32)
            rowt = P.sb("rowt", [128, ntl], F32)
            ti = P.sb("rti", [128, ntl], I32)
            P.op("dve", lambda e: e.tensor_scalar(rowt[:], post[:], 1.0 / 64, None, ALU.mult), reads=["post"], writes=["rowt"])
            P.op("dve", lambda e: e.tensor_copy(ti[:], rowt[:]), reads=["rowt"], writes=["rti"])
            P.op("dve", lambda e: e.tensor_copy(colt[:], ti[:]), reads=["rti"], writes=["colt"])
            gt = P.sb("rgt", [128, ntl], F32)
            P.op("dve", lambda e: e.tensor_tensor(gt[:], colt[:], rowt[:], ALU.is_gt), reads=["colt", "rowt"], writes=["rgt"])
            P.op("dve", lambda e: e.tensor_sub(rowt[:], colt[:], gt[:]), reads=["colt", "rgt", "rowt"], writes=["rowt"])
            P.op("dve", lambda e: e.scalar_tensor_tensor(colt[:], rowt[:], -64.0, post[:], ALU.mult, ALU.add), reads=["rowt", "post", "colt"], writes=["colt"])
            inv = P.sb("inv", [128, 16], F32)
            P.op("pool", lambda e: e.iota(inv[:], [[1, 16]], base=0, channel_multiplier=0, allow_small_or_imprecise_dtypes=True), writes=["inv"])
            P.op("act", lambda e: e.activation(out=inv[:], in_=inv[:], func=AF.Exp, scale=-math.log(10000.0) / 16), reads=["inv"], writes=["inv"])
            ang = view(scr, 0, [128, ntl, 32])
            P.op("dve", lambda e: e.tensor_tensor(ang[:, :, 0:16], rowt[:].unsqueeze(2).broadcast_to([128, ntl, 16]),
                                                  inv[:].unsqueeze(1).broadcast_to([128, ntl, 16]), ALU.mult), reads=["rowt", "inv"], writes=["ang"])
            P.op("dve", lambda e: e.tensor_tensor(ang[:, :, 16:32], colt[:].unsqueeze(2).broadcast_to([128, ntl, 16]),
                                                  inv[:].unsqueeze(1).broadcast_to([128, ntl, 16]), ALU.mult), reads=["colt", "inv"], writes=["ang"])
            sint = P.sb("sint", [128, ntl, 32], F32)
            cost = P.sb("cost", [128, ntl, 32], F32)
            rf = view(scr, ntl * 32, [128, ntl, 32])
            ri = view(scr, 2 * ntl * 32, [128, ntl, 32]).bitcast(I32)
            rm = view(scr, 3 * ntl * 32, [128, ntl, 32])
            for (dst, dk, off) in ((sint, "sint", 0.0), (cost, "cost", 0.25)):
                P.op("dve", lambda e, dst=dst, off=off: e.tensor_scalar(dst[:], ang, 1.0 / (2 * math.pi), off, ALU.mult, ALU.add), reads=["ang"], writes=[dk])
                P.op("dve", lambda e, dst=dst: e.tensor_copy(ri, dst[:]), reads=[dk], writes=["ri"])
                P.op("dve", lambda e: e.tensor_copy(rf, ri), reads=["ri"], writes=["rf"])
                P.op("dve", lambda e, dst=dst: e.tensor_sub(dst[:], dst[:], rf), reads=[dk, "rf"], writes=[dk])
                P.op("dve", lambda e, dst=dst: e.tensor_scalar(rm, dst[:], 0.5, None, ALU.is_gt), reads=[dk], writes=["rm"])
                P.op("dve", lambda e, dst=dst: e.tensor_sub(dst[:], dst[:], rm), reads=[dk, "rm"], writes=[dk])
                P.op("dve", lambda e, dst=dst: e.tensor_scalar(rm, dst[:], -0.5, None, ALU.is_lt), reads=[dk], writes=["rm"])
                P.op("dve", lambda e, dst=dst: e.tensor_add(dst[:], dst[:], rm), reads=[dk, "rm"], writes=[dk])
                P.op("act", lambda e, dst=dst: e.activation(out=dst[:], in_=dst[:], func=AF.Sin, scale=2 * math.pi), reads=[dk], writes=[dk])
            rt = [P.sb(f"rt{i}", [128, nh_rope, 32], F32) for i in range(4)]
            P.barrier()
        nrm = NormCtx(P, "n", ident, ik)
        xr = Rot(P, "x", [128, D], F32, 2)
        hT = Rot(P, "hT", [128, 8, 128], BF16, 2)
        ot = VRot("ot", [view(scr, i * N, [128, N]) for i in range(2)])
        evs = []
        nch = (N + 511) // 512
        for (r0, rows, ci) in token_tiles(n_lat, n_ctx):
            xt, xk = xr.next()
            P.dma("sp", xt[:rows], x[r0:r0 + rows, :], writes=[xk])
            G, Gk = mods[(ci, 1)]
            S, Sk = mods[(ci, 0)]
            hb, hk = nrm.norm_mod(xt, xk, rows, G, Gk, S, Sk)
            ht, htk = hT.next()
            transpose_to(P, hb, hk, rows, lambda k, ht=ht, rows=rows: ht[:, k, :rows], htk, psr, ident, ik)
            o, okk = ot.next()
            for n in range(nch):
                c0 = n * 512
                cw = min(512, N - c0)
                pt, pk = psr.next()
                for k in range(8):
                    P.op("pe", lambda e, pt=pt, ht=ht, k=k, c0=c0, cw=cw, rows=rows: e.matmul(pt[:rows, 0:cw], ht[:, k, :rows], W[:, k, c0:c0 + cw],
                                                                                   start=(k == 0), stop=(k == 7)),
                         reads=[htk, "W"], writes=[pk])
                if kind == "ml" and n == 1:
                    P.op("act", lambda e, pt=pt, o=o, c0=c0, cw=cw, rows=rows: e.mul(o[:rows, c0:c0 + cw], pt[:rows, 0:cw], ML_KSCALE), reads=[pk], writes=[okk])
                elif n % 2 == 0:
                    P.op("dve", lambda e, pt=pt, o=o, c0=c0, cw=cw, rows=rows: e.tensor_copy(o[:rows, c0:c0 + cw], pt[:rows, 0:cw]), reads=[pk], writes=[okk])
                else:
                    P.op("act", lambda e, pt=pt, o=o, c0=c0, cw=cw, rows=rows: e.copy(o[:rows, c0:c0 + cw], pt[:rows, 0:cw]), reads=[pk], writes=[okk])
            if rope and ci == 0:
                tix = r0 // 128
                ov = o[:, 0:nh_rope * 64].rearrange("p (h two j) -> p h two j", two=2, j=32)
                x1 = ov[:, :, 0, :]
                x2 = ov[:, :, 1, :]
                cb = cost[:, tix, :].unsqueeze(1).broadcast_to([128, nh_rope, 32])
                sb_ = sint[:, tix, :].unsqueeze(1).broadcast_to([128, nh_rope, 32])
                P.op("pool", lambda e, x1=x1, cb=cb: e.tensor_tensor(rt[0][:], x1, cb, ALU.mult), reads=[okk, "cost"], writes=["rt0"])
                P.op("dve", lambda e, x2=x2, sb_=sb_: e.tensor_tensor(rt[1][:], x2, sb_, ALU.mult), reads=[okk, "sint"], writes=["rt1"])
                P.op("pool", lambda e, x1=x1, sb_=sb_: e.tensor_tensor(rt[2][:], x1, sb_, ALU.mult), reads=[okk, "sint"], writes=["rt2"])
                P.op("dve", lambda e, x2=x2, cb=cb: e.tensor_tensor(rt[3][:], x2, cb, ALU.mult), reads=[okk, "cost"], writes=["rt3"])
                P.op("pool", lambda e, x1=x1: e.tensor_tensor(x1, rt[0][:], rt[1][:], ALU.subtract), reads=["rt0", "rt1", okk], writes=[okk])
                P.op("dve", lambda e, x2=x2: e.tensor_tensor(x2, rt[2][:], rt[3][:], ALU.add), reads=["rt2", "rt3", okk], writes=[okk])
            evs.append(P.dma("act", out[r0:r0 + rows, :], o[:rows, :], reads=[okk]))
        P.finish(evs)
        P.emit()
    return nc


ML_KSCALE = 64 ** -0.5


def build_C(n_lat, n_ctx, kind, ffn, last, lam_init=0.0):
    nc = bass.Bass("TRN2", target_bir_lowering=False)
    NT = n_lat + n_ctx
    x = dram_in(nc, "x", [NT, D])
    m1 = dram_in(nc, "m1", [NT, D])
    if kind == "ml":
        m2 = dram_in(nc, "m2", [NT, D])
        og = dram_in(nc, "og", [NT, D])
    if kind in ("ml", "df"):
        hn = dram_in(nc, "hn", [1, D])
    cond = dram_in(nc, "cond", [2, D])
    ada_w = dram_in(nc, "ada_w", [D, 6 * D])
    ada_b = dram_in(nc, "ada_b", [1, 6 * D])
    ng = dram_in(nc, "norm_g", [1, D])
    wo = dram_in(nc, "w_out", [D, D])
    if ffn == "dense":
        F = 2816
        NE = 1
        f_in = dram_in(nc, "f_in", [1, D, 2 * F])
        f_out = dram_in(nc, "f_out", [1, F, D])
    else:
        F = 3584
        NE = 8
        f_in = dram_in(nc, "f_in", [8, D, 2 * F])
        f_out = dram_in(nc, "f_out", [8, F, D])
        router = dram_in(nc, "router", [D, 8])
    if last:
        fg = dram_in(nc, "final_g", [1, D])
    out = dram_out(nc, "xo", [NT, D])
    f_in_bf = nc.dram_tensor("f_in_bf", [NE, D, 2 * F], BF16).ap()
    f_out_bf = nc.dram_tensor("f_out_bf", [NE, F, D], BF16).ap()
    NFC = F // 128
    FB = 2
    with contextlib.ExitStack() as st:
        P = Prog(nc, st, n_epochs=10)
        psA = Rot(P, "psA", [128, 512], F32, 4, psum=True)
        psB = Rot(P, "psB", [128, 512], F32, 3, psum=True)
        psT = Rot(P, "psT", [128, 512], F32, 1, psum=True)
        ident, ik = make_ident(P, "ident", BF16)
        mod_tiles = {(ci, j): (P.sb(f"mmod{ci}_{j}", [128, 1024], F32), f"mmod{ci}_{j}") for j in [2, 3, 4, 5] for ci in range(2)}
        scr = P.sb("scr", [128, 8192], F32)
        mods = emit_mod(P, cond, ada_w, ada_b, [2, 3, 4, 5], psA, "m", mod_tiles, scr)
        fold_gain(P, ng, mods, 4, "m")
        P.barrier()
        scr_bf = scr[:].bitcast(BF16)
        stg = VRot("stg", [scr_bf[:, i * 8192:i * 8192 + 8192] for i in range(2)])
        NJ_O = NFC // 4
        for ex in range(NE):
            for k in range(8):
                sg, sgk = stg.next()
                P.dma("pool", sg[:, 0:2 * F], f_in[ex, k * 128:(k + 1) * 128, :], writes=[sgk])
                P.dma("sp", f_in_bf[ex, k * 128:(k + 1) * 128, :], sg[:, 0:2 * F], reads=[sgk], writes=[f"finbf{ex}_{k}"])
            for q in range(0, NFC, NJ_O):
                nj = min(NJ_O, NFC - q)
                sg, sgk = stg.next()
                sv = sg[:, 0:nj * D].rearrange("p (j n) -> p j n", n=D)
                P.dma("pool", sv, f_out[ex, q * 128:(q + nj) * 128, :].rearrange("(j p) n -> p j n", p=128), writes=[sgk])
                P.dma("sp", f_out_bf[ex, q * 128:(q + nj) * 128, :].rearrange("(j p) n -> p j n", p=128), sv, reads=[sgk], writes=[f"foutbf{ex}_{q}"])
        P.barrier()
        Wo = P.sb("Wo", [128, 8, D], BF16)
        load_w_bf16(P, Wo, "Wo", wo, D)
        if kind in ("ml", "df"):
            hnb = P.sb("hnb", [128, D], F32)
            P.dma("sp", hnb[:], hn[0:1, :].partition_broadcast(128), writes=["hnb"])
            if kind == "df":
                P.op("dve", lambda e: e.tensor_scalar(hnb[:], hnb[:], 1.0 - lam_init, None, ALU.mult), reads=["hnb"], writes=["hnb"])
        if last:
            fgb = P.sb("fgb", [128, D], F32)
            P.dma("sp", fgb[:], fg[0:1, :].partition_broadcast(128), writes=["fgb"])
        if ffn == "moe":
            identf, ifk = make_ident(P, "identf", F32)
            Rt = P.sb("Rt", [128, 8, 8], F32)
            P.dma("sp", Rt[:], router.rearrange("(k p) e -> p k e", p=128), writes=["Rt"])
            h2f = Rot(P, "h2f", [128, D], F32, 1)
            h2Tf = Rot(P, "h2Tf", [128, 8, 128], F32, 1)
            gates = P.sb("gates", [128, 4, 8], F32)
        acc = view(scr, 4096, [128, 4, D])
        nrm = NormCtx(P, "n", ident, ik)
        xr = Rot(P, "x", [128, D], F32, 1)
        ar = Rot(P, "ma", [128, D], F32, 2)
        br = Rot(P, "mb", [128, D], F32, 2) if kind == "ml" else None
        yb = Rot(P, "yb", [128, D], BF16, 2)
        yT = Rot(P, "yT", [128, 8, 128], BF16, 2)
        x1 = view(scr, 0, [128, 4, D])
        h2T = P.sb("h2T", [128, 8, 512], BF16)
        hid = P.sb("hid", [128, 2 * FB, 512], BF16)
        wina = Rot(P, "wina", [128, 8, FB * 128], BF16, 2)
        winb = Rot(P, "winb", [128, 8, FB * 128], BF16, 2)
        wout = Rot(P, "wout", [128, FB, D], BF16, 2)
        sil = Rot(P, "sil", [128, 512], F32, 2)
        st8 = Rot(P, "st8", [128, 8], F32, 4)
        st1 = Rot(P, "st1", [128, 1], F32, 6)
        xo = Rot(P, "xo", [128, D], F32, 2)
        evs = []
        tiles = token_tiles(n_lat, n_ctx)
        groups = []
        lat_tiles = [t for t in tiles if t[2] == 0]
        for g0 in range(0, len(lat_tiles), 4):
            groups.append(lat_tiles[g0:g0 + 4])
        ctx_tiles = [t for t in tiles if t[2] == 1]
        if ctx_tiles:
            groups.append(ctx_tiles)
        for grp in groups:
            gw = sum(t[1] for t in grp)
            offs = []
            o_ = 0
            for t in grp:
                offs.append(o_)
                o_ += t[1]
            for ti, (r0, rows, ci) in enumerate(grp):
                xt, xk = xr.next()
                P.dma("sp", xt[:rows], x[r0:r0 + rows, :], writes=[xk])
                a, ak = ar.next()
                P.dma("sp", a[:rows], m1[r0:r0 + rows, :], writes=[ak])
                if kind == "ml":
                    b, bk = br.next()
                    P.dma("sp", b[:rows], m2[r0:r0 + rows, :], writes=[bk])
                    P.op("pool", lambda e, a=a, b=b, rows=rows: e.tensor_tensor(a[:rows], a[:rows], b[:rows], ALU.add), reads=[ak, bk], writes=[ak])
                    b, bk = br.next()
                    P.dma("sp", b[:rows], og[r0:r0 + rows, :], writes=[bk])
                    P.op("act", lambda e, b=b, rows=rows: e.activation(out=b[:rows], in_=b[:rows], func=AF.Sigmoid), reads=[bk], writes=[bk])
                y, yk = yb.next()
                if kind in ("ml", "df"):
                    sq, sqk = nrm.sq.next()
                    P.op("act", lambda e, sq=sq, a=a, rows=rows: e.activation(out=sq[:rows], in_=a[:rows], func=AF.Square), reads=[ak], writes=[sqk])
                    s8, s8k = st8.next()
                    P.op("dve", lambda e, s8=s8, sq=sq, rows=rows: e.tensor_reduce(s8[:rows], sq[:rows].rearrange("p (h v) -> p h v", v=128), AX.X, ALU.add),
                         reads=[sqk], writes=[s8k])
                    P.op("dve", lambda e, s8=s8, rows=rows: e.tensor_scalar(s8[:rows], s8[:rows], 1.0 / 128, 1e-6, ALU.mult, ALU.add), reads=[s8k], writes=[s8k])
                    P.op("act", lambda e, s8=s8, rows=rows: e.activation(out=s8[:rows], in_=s8[:rows], func=AF.Sqrt), reads=[s8k], writes=[s8k])
                    P.op("dve", lambda e, s8=s8, rows=rows: e.reciprocal(s8[:rows], s8[:rows]), reads=[s8k], writes=[s8k])
                    av = a[:rows].rearrange("p (h v) -> p h v", v=128)
                    P.op("dve", lambda e, av=av, s8=s8, rows=rows: e.tensor_tensor(av, av, s8[:rows].unsqueeze(2).broadcast_to([rows, 8, 128]), ALU.mult),
                         reads=[ak, s8k], writes=[ak])
                    if kind == "ml":
                        P.op("pool", lambda e, a=a, rows=rows: e.tensor_tensor(a[:rows], a[:rows], hnb[:rows], ALU.mult), reads=[ak, "hnb"], writes=[ak])
                        P.op("dve", lambda e, y=y, a=a, b=b, rows=rows: e.tensor_tensor(y[:rows], a[:rows], b[:rows], ALU.mult), reads=[ak, bk], writes=[yk])
                    else:
                        P.op("pool", lambda e, y=y, a=a, rows=rows: e.tensor_tensor(y[:rows], a[:rows], hnb[:rows], ALU.mult), reads=[ak, "hnb"], writes=[yk])
                else:
                    P.op("act", lambda e, y=y, a=a, rows=rows: e.copy(y[:rows], a[:rows]), reads=[ak], writes=[yk])
                yt, ytk = yT.next()
                transpose_to(P, y, yk, rows, lambda k, yt=yt, rows=rows: yt[:, k, :rows], ytk, psT, ident, ik)
                gm, gmk = mods[(ci, 2)]
                for n in range(2):
                    pt, pk = psB.next()
                    for k in range(8):
                        P.op("pe", lambda e, pt=pt, yt=yt, k=k, n=n, rows=rows: e.matmul(pt[:rows, :], yt[:, k, :rows], Wo[:, k, n * 512:(n + 1) * 512],
                                                                                         start=(k == 0), stop=(k == 7)),
                             reads=[ytk, "Wo"], writes=[pk])
                    P.op("dve", lambda e, pt=pt, n=n, ti=ti, rows=rows, gm=gm: e.tensor_tensor(x1[:rows, ti, n * 512:(n + 1) * 512], pt[:rows, :], gm[:rows, n * 512:(n + 1) * 512], ALU.mult),
                         reads=[pk, gmk], writes=[f"x1_{ti}"])
                P.op("pool", lambda e, ti=ti, rows=rows, xt=xt: e.tensor_tensor(x1[:rows, ti, :], x1[:rows, ti, :], xt[:rows], ALU.add), reads=[f"x1_{ti}", xk], writes=[f"x1_{ti}"])
                G, Gk = mods[(ci, 4)]
                S, Sk = mods[(ci, 3)]
                if ffn == "moe":
                    hf, hfk = h2f.next()
                    hb, hk = nrm.norm_mod(x1[:, ti, :], f"x1_{ti}", rows, G, Gk, S, Sk, out_f32=(hf, hfk))
                else:
                    hb, hk = nrm.norm_mod(x1[:, ti, :], f"x1_{ti}", rows, G, Gk, S, Sk)
                o0 = offs[ti]
                transpose_to(P, hb, hk, rows, lambda k, o0=o0, rows=rows: h2T[:, k, o0:o0 + rows], "h2T", psT, ident, ik)
                if ffn == "moe":
                    htf, htfk = h2Tf.next()
                    transpose_to(P, hf, hfk, rows, lambda k, htf=htf, rows=rows: htf[:, k, :rows], htfk, psT, identf, ifk, dt=F32, evac="dve")
                    pt, pk = psB.next()
                    for k in range(8):
                        P.op("pe", lambda e, pt=pt, htf=htf, k=k, rows=rows: e.matmul(pt[:rows, 0:8], htf[:, k, :rows], Rt[:, k, :], start=(k == 0), stop=(k == 7)),
                             reads=[htfk, "Rt"], writes=[pk])
                    lg, lgk = st8.next()
                    P.op("dve", lambda e, lg=lg, pt=pt, rows=rows: e.tensor_copy(lg[:rows], pt[:rows, 0:8]), reads=[pk], writes=[lgk])
                    mx1, mx1k = st1.next()
                    P.op("dve", lambda e, mx1=mx1, lg=lg, rows=rows: e.tensor_reduce(mx1[:rows], lg[:rows], AX.X, ALU.max), reads=[lgk], writes=[mx1k])
                    eq, eqk = st8.next()
                    P.op("dve", lambda e, eq=eq, lg=lg, mx1=mx1, rows=rows: e.tensor_scalar(eq[:rows], lg[:rows], mx1[:rows, 0:1], None, ALU.is_equal), reads=[lgk, mx1k], writes=[eqk])
                    P.op("dve", lambda e, eq=eq, lg=lg, rows=rows: e.scalar_tensor_tensor(eq[:rows], eq[:rows], -1e30, lg[:rows], ALU.mult, ALU.add), reads=[eqk, lgk], writes=[eqk])
                    mx2, mx2k = st1.next()
                    P.op("dve", lambda e, mx2=mx2, eq=eq, rows=rows: e.tensor_reduce(mx2[:rows], eq[:rows], AX.X, ALU.max), reads=[eqk], writes=[mx2k])
                    P.op("dve", lambda e, eq=eq, lg=lg, mx2=mx2, rows=rows: e.tensor_scalar(eq[:rows], lg[:rows], mx2[:rows, 0:1], None, ALU.is_ge), reads=[lgk, mx2k, eqk], writes=[eqk])
                    nm, nmk = st1.next()
                    P.op("dve", lambda e, nm=nm, mx1=mx1, rows=rows: e.tensor_scalar(nm[:rows], mx1[:rows], -1.0, None, ALU.mult), reads=[mx1k], writes=[nmk])
                    P.op("act", lambda e, lg=lg, nm=nm, rows=rows: e.activation(out=lg[:rows], in_=lg[:rows], func=AF.Exp, bias=nm[:rows, 0:1]), reads=[lgk, nmk], writes=[lgk])
                    P.op("dve", lambda e, lg=lg, eq=eq, rows=rows: e.tensor_tensor(lg[:rows], lg[:rows], eq[:rows], ALU.mult), reads=[lgk, eqk], writes=[lgk])
                    P.op("dve", lambda e, mx2=mx2, lg=lg, rows=rows: e.tensor_reduce(mx2[:rows], lg[:rows], AX.X, ALU.add), reads=[lgk, mx2k], writes=[mx2k])
                    P.op("dve", lambda e, mx2=mx2, rows=rows: e.reciprocal(mx2[:rows], mx2[:rows]), reads=[mx2k], writes=[mx2k])
                    P.op("dve", lambda e, lg=lg, mx2=mx2, ti=ti, rows=rows: e.tensor_scalar(gates[:rows, ti, :], lg[:rows], mx2[:rows, 0:1], None, ALU.mult), reads=[lgk, mx2k], writes=[f"gates{ti}"])
            for ex in range(NE):
                for fb in range(NFC // FB):
                    wa, wak = wina.next()
                    wb, wbk = winb.next()
                    wo2, wo2k = wout.next()
                    f0 = fb * FB * 128
                    fin_v = f_in_bf[ex].rearrange("(k p) n -> p k n", p=128)
                    P.dma("sp", wa[:], fin_v[:, :, f0:f0 + FB * 128], writes=[wak])
                    P.dma("sp", wb[:], fin_v[:, :, F + f0:F + f0 + FB * 128], writes=[wbk])
                    P.dma("sp", wo2[:], f_out_bf[ex, f0:f0 + FB * 128, :].rearrange("(j p) n -> p j n", p=128), writes=[wo2k])
                    for j in range(FB):
                        fc = (fb % 2) * FB + j
                        pa, pak = psA.next()
                        pb, pbk = psA.next()
                        for k in range(8):
                            P.op("pe", lambda e, pa=pa, wa=wa, k=k, j=j, gw=gw: e.matmul(pa[:, 0:gw], wa[:, k, j * 128:(j + 1) * 128], h2T[:, k, 0:gw], start=(k == 0), stop=(k == 7)),
                                 reads=[wak, "h2T"], writes=[pak])
                        for k in range(8):
                            P.op("pe", lambda e, pb=pb, wb=wb, k=k, j=j, gw=gw: e.matmul(pb[:, 0:gw], wb[:, k, j * 128:(j + 1) * 128], h2T[:, k, 0:gw], start=(k == 0), stop=(k == 7)),
                                 reads=[wbk, "h2T"], writes=[pbk])
                        sl, slk = sil.next()
                        P.op("act", lambda e, sl=sl, pa=pa, gw=gw: e.activation(out=sl[:, 0:gw], in_=pa[:, 0:gw], func=AF.Silu), reads=[pak], writes=[slk])
                        P.op("dve", lambda e, sl=sl, pb=pb, fc=fc, gw=gw: e.tensor_tensor(hid[:, fc, 0:gw], sl[:, 0:gw], pb[:, 0:gw], ALU.mult), reads=[slk, pbk], writes=[f"hid{fc}"])
                    for ti, (r0, rows, ci) in enumerate(grp):
                        o0 = offs[ti]
                        for n in range(2):
                            pt, pk = psB.next()
                            for j in range(FB):
                                fc = (fb % 2) * FB + j
                                P.op("pe", lambda e, pt=pt, fc=fc, j=j, n=n, o0=o0, rows=rows, wo2=wo2: e.matmul(pt[:rows, :], hid[:, fc, o0:o0 + rows], wo2[:, j, n * 512:(n + 1) * 512],
                                                                                                              start=(j == 0), stop=(j == FB - 1)),
                                     reads=[f"hid{fc}", wo2k], writes=[pk])
                            first = (ex == 0 and fb == 0)
                            if ffn == "moe":
                                gcol = gates[:rows, ti, ex:ex + 1]
                                if first:
                                    P.op("dve", lambda e, pt=pt, ti=ti, n=n, rows=rows, gcol=gcol: e.tensor_scalar(acc[:rows, ti, n * 512:(n + 1) * 512], pt[:rows, :], gcol, None, ALU.mult),
                                         reads=[pk, f"gates{ti}"], writes=[f"acc{ti}_{n}"])
                                else:
                                    P.op("dve", lambda e, pt=pt, ti=ti, n=n, rows=rows, gcol=gcol: e.scalar_tensor_tensor(acc[:rows, ti, n * 512:(n + 1) * 512], pt[:rows, :], gcol,
                                                                                                                          acc[:rows, ti, n * 512:(n + 1) * 512], ALU.mult, ALU.add),
                                         reads=[pk, f"gates{ti}", f"acc{ti}_{n}"], writes=[f"acc{ti}_{n}"])
                            else:
                                if first:
                                    P.op("dve", lambda e, pt=pt, ti=ti, n=n, rows=rows: e.tensor_copy(acc[:rows, ti, n * 512:(n + 1) * 512], pt[:rows, :]),
                                         reads=[pk], writes=[f"acc{ti}_{n}"])
                                else:
                                    P.op("dve", lambda e, pt=pt, ti=ti, n=n, rows=rows: e.tensor_tensor(acc[:rows, ti, n * 512:(n + 1) * 512], pt[:rows, :], acc[:rows, ti, n * 512:(n + 1) * 512], ALU.add),
                                         reads=[pk, f"acc{ti}_{n}"], writes=[f"acc{ti}_{n}"])
            A_ = acc
            for ti, (r0, rows, ci) in enumerate(grp):
                gl, glk = mods[(ci, 5)]
                xot, xok = xo.next()
                P.op("dve", lambda e, xot=xot, ti=ti, rows=rows, gl=gl: e.tensor_tensor(xot[:rows], A_[:rows, ti, :], gl[:rows], ALU.mult),
                     reads=[f"acc{ti}_0", f"acc{ti}_1", glk], writes=[xok])
                P.op("pool", lambda e, xot=xot, ti=ti, rows=rows: e.tensor_tensor(xot[:rows], xot[:rows], x1[:rows, ti, :], ALU.add), reads=[xok, f"x1_{ti}"], writes=[xok])
                if last:
                    sq, sqk = nrm.sq.next()
                    ss, ssk = st1.next()
                    P.op("act", lambda e, sq=sq, xot=xot, ss=ss, rows=rows: e.activation(out=sq[:rows], in_=xot[:rows], func=AF.Square, accum_out=ss[:rows]), reads=[xok], writes=[sqk, ssk])
                    P.op("dve", lambda e, ss=ss, rows=rows: e.tensor_scalar(ss[:rows], ss[:rows], 1.0 / D, 1e-6, ALU.mult, ALU.add), reads=[ssk], writes=[ssk])
                    P.op("act", lambda e, ss=ss, rows=rows: e.activation(out=ss[:rows], in_=ss[:rows], func=AF.Sqrt), reads=[ssk], writes=[ssk])
                    P.op("dve", lambda e, ss=ss, rows=rows: e.reciprocal(ss[:rows], ss[:rows]), reads=[ssk], writes=[ssk])
                    P.op("dve", lambda e, xot=xot, ss=ss, rows=rows: e.scalar_tensor_tensor(xot[:rows], xot[:rows], ss[:rows, 0:1], fgb[:rows], ALU.mult, ALU.mult), reads=[xok, ssk, "fgb"], writes=[xok])
                evs.append(P.dma("act", out[r0:r0 + rows, :], xot[:rows, :], reads=[xok]))
        P.finish(evs)
        P.emit()
    return nc


def build_B_ml(T, GC=5):
    NJ = 4
    NCH = T // 128
    NSEG = NCH // GC
    assert NSEG * GC == NCH
    R = NJ * NSEG
    GW = GC * 128
    nc = bass.Bass("TRN2", target_bir_lowering=False)
    qT = dram_in(nc, "qT", [NJ, 64, T])
    kT = dram_in(nc, "kT", [NJ, 64, T])
    ktok = dram_in(nc, "ktok", [NJ, T, 64])
    vtok = dram_in(nc, "vtok", [NJ, T, 128])
    ipre = dram_in(nc, "ipre", [R, GW])
    fpre = dram_in(nc, "fpre", [R, GW])
    gbias = dram_in(nc, "gbias", [R, 2])
    hout = dram_out(nc, "h", [NJ, T, 128])
    s_be = nc.dram_tensor("s_be", [R, GC], F32).ap()
    s_ml = nc.dram_tensor("s_ml", [R, GC], F32).ap()
    s_mi = nc.dram_tensor("s_mi", [NJ, NCH], F32).ap()
    with contextlib.ExitStack() as st:
        P = Prog(nc, st, n_epochs=8)
        identf, ifk = make_ident(P, "identf", F32)
        maskU, muk = make_tri(P, "maskU", True)
        it = P.sb("g_it", [R, GW], F32)
        ft = P.sb("g_ft", [R, GW], F32)
        gb = P.sb("g_gb", [R, 2], F32)
        P.dma("sp", it[:], ipre, writes=["it"])
        P.dma("sp", ft[:], fpre, writes=["ft"])
        P.dma("sp", gb[:], gbias, writes=["gb"])
        P.op("dve", lambda e: e.tensor_scalar(it[:], it[:], gb[:, 0:1], None, ALU.add), reads=["it", "gb"], writes=["it"])
        P.op("dve", lambda e: e.tensor_scalar(ft[:], ft[:], gb[:, 1:2], None, ALU.add), reads=["ft", "gb"], writes=["ft"])
        P.op("act", lambda e: e.activation(out=ft[:], in_=ft[:], func=AF.Exp, scale=-1.0), reads=["ft"], writes=["ft"])
        P.op("act", lambda e: e.activation(out=ft[:], in_=ft[:], func=AF.Ln, bias=1.0), reads=["ft"], writes=["ft"])
        rmask = P.sb("g_rm", [R, GW], F32)
        nmask = P.sb("g_nm", [R, GW], F32)
        P.op("dve", lambda e: e.memset(rmask[:], 1.0), writes=["rmask"])
        P.op("dve", lambda e: e.memset(rmask[:, 0:GW:128], 0.0), reads=["rmask"], writes=["rmask"])
        P.op("dve", lambda e: e.memset(nmask[:], 0.0), writes=["nmask"])
        P.op("dve", lambda e: e.memset(nmask[:, 0:GW:128], -1e30), reads=["nmask"], writes=["nmask"])
        nb = P.sb("g_nb", [R, GW], F32)
        P.op("dve", lambda e: e.tensor_tensor_scan(nb[:], rmask[:], ft[:], 0.0, ALU.mult, ALU.add), reads=["rmask", "ft"], writes=["nb"])
        u = P.sb("g_u", [R, GW], F32)
        P.op("dve", lambda e: e.tensor_tensor(u[:], it[:], nb[:], ALU.add), reads=["it", "nb"], writes=["u"])
        cu = P.sb("g_cu", [R, GW], F32)
        P.op("dve", lambda e: e.tensor_tensor_scan(cu[:], nmask[:], u[:], 0.0, ALU.add, ALU.max), reads=["nmask", "u"], writes=["cu"])
        cmax = P.sb("g_cmax", [R, GC], F32)
        bend = P.sb("g_bend", [R, GC], F32)
        mloc = P.sb("g_mloc", [R, GC], F32)
        P.op("dve", lambda e: e.tensor_copy(cmax[:], cu[:, 127:GW:128]), reads=["cu"], writes=["cmax"])
        P.op("dve", lambda e: e.tensor_scalar(bend[:], nb[:, 127:GW:128], -1.0, None, ALU.mult), reads=["nb"], writes=["bend"])
        P.op("dve", lambda e: e.tensor_tensor(mloc[:], bend[:], cmax[:], ALU.add), reads=["bend", "cmax"], writes=["mloc"])
        E = P.sb("g_E", [R, GW], F32)
        P.op("dve", lambda e: e.tensor_tensor(E[:].rearrange("r (c t) -> r c t", t=128), u[:].rearrange("r (c t) -> r c t", t=128),
                                              cmax[:].unsqueeze(2).broadcast_to([R, GC, 128]), ALU.subtract), reads=["u", "cmax"], writes=["E"])
        P.op("act", lambda e: e.activation(out=E[:], in_=E[:], func=AF.Exp), reads=["E"], writes=["E"])
        e1 = P.dma("sp", s_be, bend[:], reads=["bend"], writes=["s_be"])
        e2 = P.dma("sp", s_ml, mloc[:], reads=["mloc"], writes=["s_ml"])
        be4 = P.sb("g_be4", [NJ, NCH], F32)
        ml4 = P.sb("g_ml4", [NJ, NCH], F32)
        P.dma("sp", be4[:], s_be.rearrange("(j s) c -> j (s c)", j=NJ), reads=["s_be"], writes=["be4"])
        P.dma("sp", ml4[:], s_ml.rearrange("(j s) c -> j (s c)", j=NJ), reads=["s_ml"], writes=["ml4"])
        mo4 = P.sb("g_mo4", [NJ, NCH], F32)
        mi4 = P.sb("g_mi4", [NJ, NCH], F32)
        P.op("dve", lambda e: e.tensor_tensor_scan(mo4[:], be4[:], ml4[:], 0.0, ALU.add, ALU.max), reads=["be4", "ml4"], writes=["mo4"])
        P.op("dve", lambda e: e.memset(mi4[:, 0:1], 0.0), writes=["mi4"])
        if NCH > 1:
            P.op("dve", lambda e: e.tensor_copy(mi4[:, 1:NCH], mo4[:, 0:NCH - 1]), reads=["mo4", "mi4"], writes=["mi4"])
        as4 = P.sb("g_as4", [NJ, 2, NCH], F32)
        P.op("dve", lambda e: e.tensor_tensor(as4[:, 0, :], be4[:], mi4[:], ALU.add), reads=["be4", "mi4"], writes=["as4"])
        P.op("dve", lambda e: e.tensor_tensor(as4[:, 0, :], as4[:, 0, :], mo4[:], ALU.subtract), reads=["as4", "mo4"], writes=["as4"])
        P.op("dve", lambda e: e.tensor_tensor(as4[:, 1, :], ml4[:], mo4[:], ALU.subtract), reads=["ml4", "mo4", "as4"], writes=["as4"])
        P.op("act", lambda e: e.activation(out=as4[:], in_=as4[:], func=AF.Exp), reads=["as4"], writes=["as4"])
        ASb = P.sb("g_ASb", [64, NJ, 2, NCH], F32)
        sel = P.sb("g_sel", [NJ, NJ, 64], F32)
        P.op("pool", lambda e: e.memset(sel[:], 0.0), writes=["sel"])
        for j in range(NJ):
            P.op("pool", lambda e, j=j: e.affine_select(out=sel[:, j, :], in_=sel[:, j, :], pattern=[[0, 64]], compare_op=ALU.not_equal,
                                                        fill=1.0, base=-j, channel_multiplier=1), reads=["sel"], writes=["sel"])
        psG = Rot(P, "psG", [128, 512], F32, 2, psum=True)
        for j in range(NJ):
            pt, pk = psG.next()
            P.op("pe", lambda e, pt=pt, j=j: e.matmul(pt[0:64, 0:2 * NCH], sel[:, j, :], as4[:].rearrange("j a c -> j (a c)"), start=True, stop=True),
                 reads=["sel", "as4"], writes=[pk])
            P.op("dve", lambda e, pt=pt, j=j: e.tensor_copy(ASb[:, j, :, :].rearrange("p a c -> p (a c)"), pt[0:64, 0:2 * NCH]), reads=[pk], writes=["ASb"])
        P.dma("sp", s_mi, mi4[:], reads=["mi4"], writes=["s_mi"])
        mi = P.sb("g_mi", [R, GC], F32)
        P.dma("sp", mi[:], s_mi.rearrange("j (s c) -> (j s) c", c=GC), reads=["s_mi"], writes=["mi"])
        M = P.sb("g_M", [R, GW], F32)
        Mv = M[:].rearrange("r (c t) -> r c t", t=128)
        P.op("dve", lambda e: e.tensor_tensor(Mv, cu[:].rearrange("r (c t) -> r c t", t=128), mi[:].unsqueeze(2).broadcast_to([R, GC, 128]), ALU.max),
             reads=["cu", "mi"], writes=["M"])
        P.op("dve", lambda e: e.tensor_tensor(cu[:].rearrange("r (c t) -> r c t", t=128), cmax[:].unsqueeze(2).broadcast_to([R, GC, 128]), Mv, ALU.subtract),
             reads=["cmax", "M", "cu"], writes=["cu"])
        P.op("act", lambda e: e.activation(out=cu[:], in_=cu[:], func=AF.Exp), reads=["cu"], writes=["cu"])
        P.op("dve", lambda e: e.tensor_tensor(u[:].rearrange("r (c t) -> r c t", t=128), mi[:].unsqueeze(2).broadcast_to([R, GC, 128]), Mv, ALU.subtract),
             reads=["mi", "M", "u"], writes=["u"])
        P.op("act", lambda e: e.activation(out=u[:], in_=u[:], func=AF.Exp), reads=["u"], writes=["u"])
        P.op("dve", lambda e: e.tensor_tensor(nb[:], nb[:], M[:], ALU.subtract), reads=["nb", "M"], writes=["nb"])
        P.op("act", lambda e: e.activation(out=nb[:], in_=nb[:], func=AF.Exp), reads=["nb"], writes=["nb"])
        TM = P.sb("g_TM", [128, 4, GC, R], F32)
        for qi, (src, sk) in enumerate(((E, "E"), (cu, "cu"), (u, "u"), (nb, "nb"))):
            for j in range(GC):
                pt, pk = psG.next()
                P.op("pe", lambda e, pt=pt, src=src, j=j: e.transpose(pt[:, 0:R], src[:, j * 128:(j + 1) * 128], identf[:R, :R]), reads=[sk, ifk], writes=[pk])
                P.op("dve", lambda e, pt=pt, qi=qi, j=j: e.tensor_copy(TM[:, qi, j, :], pt[:, 0:R]), reads=[pk], writes=["TM"])
        P.barrier()
        Cst = [P.sb(f"Cst{j}", [64, 129], F32) for j in range(NJ)]
        Cbf = [P.sb(f"Cbf{j}", [64, 129], BF16) for j in range(NJ)]
        for j in range(NJ):
            P.op("dve", lambda e, j=j: e.memset(Cst[j][:], 0.0), writes=[f"Cst{j}"])
            P.op("pool", lambda e, j=j: e.memset(Cbf[j][:], 0.0), writes=[f"Cbf{j}"])
        NB_ = 2
        qg = [Rot(P, f"qg{j}_", [64, GW], BF16, NB_) for j in range(NJ)]
        kg = [Rot(P, f"kg{j}_", [64, GW], BF16, NB_) for j in range(NJ)]
        ktg = [Rot(P, f"ktg{j}_", [128, GC, 64], BF16, NB_) for j in range(NJ)]
        vg = [Rot(P, f"vg{j}_", [128, GC, 129], BF16, NB_) for j in range(NJ)]
        hg = [Rot(P, f"hg{j}_", [128, GC, 128], F32, NB_) for j in range(NJ)]
        for j in range(NJ):
            for (t, k) in zip(vg[j].tiles, vg[j].keys):
                P.op("pool", lambda e, t=t: e.memset(t[:, :, 128:129], 1.0), writes=[k])
        psS = Rot(P, "psS", [128, 512], F32, 2, psum=True)
        psO = Rot(P, "psO", [128, 512], F32, 2, psum=True)
        psC = Rot(P, "psC", [128, 512], F32, 2, psum=True)
        Sm = Rot(P, "Sm", [128, 128], BF16, 3)
        ev = Rot(P, "ev", [128, 129], BF16, 3)
        n1 = Rot(P, "n1", [128, 129], F32, 3)
        dn = Rot(P, "dn", [128, 1], F32, 4)
        tC = Rot(P, "tC", [64, 129], F32, 2)
        evs = []
        for seg in range(NSEG):
            t0 = seg * GW
            cur = []
            for j in range(NJ):
                q_, qk = qg[j].next()
                k_, kk = kg[j].next()
                kt_, ktk = ktg[j].next()
                v_, vk = vg[j].next()
                h_, hk = hg[j].next()
                P.dma("pool", q_[:], qT[j, :, t0:t0 + GW], writes=[qk])
                P.dma("pool", k_[:], kT[j, :, t0:t0 + GW], writes=[kk])
                P.dma("pool", kt_[:], ktok[j, t0:t0 + GW, :].rearrange("(c p) d -> p c d", p=128), writes=[ktk])
                P.dma("pool", v_[:, :, 0:128], vtok[j, t0:t0 + GW, :].rearrange("(c p) d -> p c d", p=128), writes=[vk])
                cur.append((q_, qk, k_, kk, kt_, ktk, v_, vk, h_, hk))
            for c in range(GC):
                cg = seg * GC + c
                for j in range(NJ):
                    q_, qk, k_, kk, kt_, ktk, v_, vk, h_, hk = cur[j]
                    r = j * NSEG + seg
                    cs = slice(c * 128, (c + 1) * 128)
                    pS, pSk = psS.next()
                    P.op("pe", lambda e, pS=pS, k_=k_, q_=q_, cs=cs: e.matmul(pS[:, 0:128], k_[:, cs], q_[:, cs], start=True, stop=True), reads=[kk, qk], writes=[pSk])
                    sm, smk = Sm.next()
                    P.op("dve", lambda e, sm=sm, pS=pS: e.tensor_tensor(sm[:], pS[:, 0:128], maskU[:], ALU.mult), reads=[pSk, muk], writes=[smk])
                    e_, ek = ev.next()
                    P.op("act", lambda e, e_=e_, v_=v_, c=c, r=r: e.activation(out=e_[:], in_=v_[:, c, :], func=AF.Copy, scale=TM[:, 0, c, r:r + 1]), reads=[vk, "TM"], writes=[ek])
                    pO, pOk = psO.next()
                    P.op("pe", lambda e, pO=pO, sm=sm, e_=e_: e.matmul(pO[:, 0:129], sm[:], e_[:], start=True, stop=True), reads=[smk, ek], writes=[pOk])
                    P.op("pe", lambda e, pO=pO, q_=q_, cs=cs, j=j: e.matmul(pO[:, 256:385], q_[:, cs], Cbf[j][:], start=True, stop=True), reads=[qk, f"Cbf{j}"], writes=[pOk])
                    n_, nk = n1.next()
                    P.op("dve", lambda e, n_=n_, pO=pO, c=c, r=r: e.tensor_scalar(n_[:], pO[:, 0:129], TM[:, 1, c, r:r + 1], None, ALU.mult), reads=[pOk, "TM"], writes=[nk])
                    P.op("dve", lambda e, n_=n_, pO=pO, c=c, r=r: e.scalar_tensor_tensor(n_[:], pO[:, 256:385], TM[:, 2, c, r:r + 1], n_[:], ALU.mult, ALU.add),
                         reads=[pOk, "TM", nk], writes=[nk])
                    d_, dk = dn.next()
                    P.op("act", lambda e, d_=d_, n_=n_: e.activation(out=d_[:], in_=n_[:, 128:129], func=AF.Abs), reads=[nk], writes=[dk])
                    P.op("dve", lambda e, d_=d_, c=c, r=r: e.tensor_tensor(d_[:], d_[:], TM[:, 3, c, r:r + 1], ALU.max), reads=[dk, "TM"], writes=[dk])
                    P.op("dve", lambda e, d_=d_: e.reciprocal(d_[:], d_[:]), reads=[dk], writes=[dk])
                    P.op("act", lambda e, h_=h_, n_=n_, d_=d_, c=c: e.activation(out=h_[:, c, :], in_=n_[:, 0:128], func=AF.Copy, scale=d_[:, 0:1]), reads=[nk, dk], writes=[hk])
                    pC, pCk = psC.next()
                    P.op("pe", lambda e, pC=pC, kt_=kt_, e_=e_, c=c: e.matmul(pC[0:64, 0:129], kt_[:, c, :], e_[:], start=True, stop=True), reads=[ktk, ek], writes=[pCk])
                    t_, tk = tC.next()
                    P.op("dve", lambda e, t_=t_, pC=pC, j=j, cg=cg: e.tensor_scalar(t_[:], pC[0:64, 0:129], ASb[:, j, 1, cg:cg + 1], None, ALU.mult), reads=[pCk, "ASb"], writes=[tk])
                    P.op("dve", lambda e, t_=t_, j=j, cg=cg: e.scalar_tensor_tensor(Cst[j][:], Cst[j][:], ASb[:, j, 0, cg:cg + 1], t_[:], ALU.mult, ALU.add),
                         reads=[f"Cst{j}", "ASb", tk], writes=[f"Cst{j}"])
                    P.op("act", lambda e, j=j: e.copy(Cbf[j][:], Cst[j][:]), reads=[f"Cst{j}"], writes=[f"Cbf{j}"])
            for j in range(NJ):
                h_, hk = cur[j][8], cur[j][9]
                evs.append(P.dma("sp", hout[j, t0:t0 + GW, :].rearrange("(c p) d -> p c d", p=128), h_[:], reads=[hk]))
        P.finish(evs)
        P.emit()
    return nc


def build_B_df(T, NCX, lam_init):
    NP = 2
    NCH = T // 128
    nc = bass.Bass("TRN2", target_bir_lowering=False)
    qT = dram_in(nc, "qT", [NP, 64, 2, T])
    kT = dram_in(nc, "kT", [NP, 64, 2, T])
    vt = dram_in(nc, "v", [NP, T, 128])
    lam = dram_in(nc, "lam", [1, 256])
    out = dram_out(nc, "o", [NP, T, 128])
    with contextlib.ExitStack() as st:
        P = Prog(nc, st, n_epochs=8)
        lb = P.sb("lb", [128, 4, 64], F32)
        P.dma("sp", lb[:].rearrange("p a b -> p (a b)"), lam[0:1, :].partition_broadcast(128), writes=["lb"])
        lt = P.sb("lt", [128, 2, 64], F32)
        ls = P.sb("ls", [128, 2], F32)
        nlam = P.sb("nlam", [128, 1], F32)
        P.op("dve", lambda e: e.tensor_tensor(lt[:, 0, :], lb[:, 0, :], lb[:, 1, :], ALU.mult), reads=["lb"], writes=["lt"])
        P.op("dve", lambda e: e.tensor_tensor(lt[:, 1, :], lb[:, 2, :], lb[:, 3, :], ALU.mult), reads=["lb", "lt"], writes=["lt"])
        P.op("dve", lambda e: e.tensor_reduce(ls[:], lt[:], AX.X, ALU.add), reads=["lt"], writes=["ls"])
        P.op("act", lambda e: e.activation(out=ls[:], in_=ls[:], func=AF.Exp), reads=["ls"], writes=["ls"])
        P.op("dve", lambda e: e.tensor_tensor(nlam[:], ls[:, 1:2], ls[:, 0:1], ALU.subtract), reads=["ls"], writes=["nlam"])
        P.op("dve", lambda e: e.tensor_scalar(nlam[:], nlam[:], -lam_init, None, ALU.add), reads=["nlam"], writes=["nlam"])
        k12 = P.sb("k12", [64, 2, T], BF16)
        va = P.sb("va", [128, NCH, 129], BF16)
        P.op("pool", lambda e: e.memset(va[:, :, 128:129], 1.0), writes=["va"])
        q12 = Rot(P, "q12_", [64, 2, 512], BF16, 2)
        pex = Rot(P, "pex", [128, 512], BF16, 4)
        psS = Rot(P, "psS", [128, 512], F32, 4, psum=True)
        psO = [P.ps(f"psO{i}", [128, 512], F32) for i in range(4)]
        ot = Rot(P, "ot", [128, 4, 128], F32, 2)
        t1 = Rot(P, "t1_", [128, 128], F32, 2)
        rd = Rot(P, "rd", [128, 2], F32, 4)
        evs = []
        qtiles = [(0, NCX, NCX // 128)]
        q0 = NCX
        while q0 < T:
            qw = min(512, T - q0)
            qtiles.append((q0, qw, NCH))
            q0 += qw
        CW = 2048
        for p in range(NP):
            for c0 in range(0, T, CW):
                cw = min(CW, T - c0)
                P.dma("pool", k12[:, :, c0:c0 + cw], kT[p, :, :, c0:c0 + cw], writes=["k12"])
                P.dma("pool", va[:, c0 // 128:(c0 + cw) // 128, 0:128], vt[p, c0:c0 + cw, :].rearrange("(c q) d -> q c d", q=128), writes=["va"])
            for (q0, qw, nk) in qtiles:
                qq, qk = q12.next()
                P.dma("pool", qq[:, :, 0:qw], qT[p, :, :, q0:q0 + qw], writes=[qk])
                nq = qw // 128
                for kc in range(nk):
                    pes = []
                    for sub in range(2):
                        pS, pSk = psS.next()
                        P.op("pe", lambda e, pS=pS, sub=sub, kc=kc, qq=qq, qw=qw: e.matmul(pS[:, 0:qw], k12[:, sub, kc * 128:(kc + 1) * 128],
                                                                                         qq[:, sub, 0:qw], start=True, stop=True),
                             reads=["k12", qk], writes=[pSk])
                        pe_, pek = pex.next()
                        P.op("act", lambda e, pe_=pe_, pS=pS, qw=qw: e.activation(out=pe_[:, 0:qw], in_=pS[:, 0:qw], func=AF.Exp, scale=0.125), reads=[pSk], writes=[pek])
                        pes.append((pe_, pek))
                    for qi in range(nq):
                        for sub in range(2):
                            pe_, pek = pes[sub]
                            P.op("pe", lambda e, qi=qi, sub=sub, pe_=pe_, kc=kc, nk=nk: e.matmul(psO[qi][:, sub * 256:sub * 256 + 129], pe_[:, qi * 128:(qi + 1) * 128], va[:, kc, :],
                                                                                               start=(kc == 0 and sub == 0), stop=(kc == nk - 1), skip_group_check=True),
                                 reads=[pek, "va"], writes=[f"psO{qi}"])
                o_, ok_ = ot.next()
                for qi in range(nq):
                    r_, rk = rd.next()
                    P.op("dve", lambda e, r_=r_, qi=qi: e.reciprocal(r_[:, 0:1], psO[qi][:, 128:129]), reads=[f"psO{qi}"], writes=[rk])
                    P.op("dve", lambda e, r_=r_, qi=qi: e.reciprocal(r_[:, 1:2], psO[qi][:, 256 + 128:256 + 129]), reads=[f"psO{qi}", rk], writes=[rk])
                    P.op("dve", lambda e, r_=r_: e.tensor_tensor(r_[:, 1:2], r_[:, 1:2], nlam[:], ALU.mult), reads=[rk, "nlam"], writes=[rk])
                    t_, tk = t1.next()
                    P.op("dve", lambda e, t_=t_, r_=r_, qi=qi: e.tensor_scalar(t_[:], psO[qi][:, 0:128], r_[:, 0:1], None, ALU.mult), reads=[f"psO{qi}", rk], writes=[tk])
                    P.op("dve", lambda e, t_=t_, r_=r_, qi=qi, o_=o_: e.scalar_tensor_tensor(o_[:, qi, :], psO[qi][:, 256:384], r_[:, 1:2], t_[:], ALU.mult, ALU.add),
                         reads=[f"psO{qi}", rk, tk], writes=[ok_])
                evs.append(P.dma("sp", out[p, q0:q0 + qw, :].rearrange("(c q) d -> q c d", q=128), o_[:, 0:nq, :], reads=[ok_]))
        P.finish(evs)
        P.emit()
    return nc


def build_B_sw(T, NCX):
    NBLK = T // 128
    NCB = NCX // 128
    NLB = NBLK - NCB
    nc = bass.Bass("TRN2", target_bir_lowering=False)
    qT = dram_in(nc, "qT", [64, NBLK, 512])
    kT = dram_in(nc, "kT", [64, T])
    vt = dram_in(nc, "v", [T, 64])
    sinks = dram_in(nc, "sinks", [1, 4])
    out = dram_out(nc, "o", [T, 256])
    with contextlib.ExitStack() as st:
        P = Prog(nc, st, n_epochs=6)
        maskU, muk = make_tri(P, "maskU", True, BF16)
        maskL, mlk = make_tri(P, "maskL", False, BF16)
        es = P.sb("es", [128, 4], F32)
        P.dma("sp", es[:], sinks[0:1, :].partition_broadcast(128), writes=["es"])
        P.op("act", lambda e: e.activation(out=es[:], in_=es[:], func=AF.Exp), reads=["es"], writes=["es"])
        kk = P.sb("kk", [64, T], BF16)
        va = P.sb("va", [128, NBLK, 65], BF16)
        P.op("pool", lambda e: e.memset(va[:, :, 64:65], 1.0), writes=["va"])
        CW = 2048
        for c0 in range(0, T, CW):
            cw = min(CW, T - c0)
            P.dma("pool", kk[:, c0:c0 + cw], kT[:, c0:c0 + cw], writes=["kk"])
            P.dma("pool", va[:, c0 // 128:(c0 + cw) // 128, 0:64], vt[c0:c0 + cw, :].rearrange("(c q) d -> q c d", q=128), writes=["va"])
        qb = Rot(P, "qb", [64, 512], BF16, 3)
        psS = Rot(P, "psS", [128, 512], F32, 6, psum=True)
        psO = Rot(P, "psO", [128, 512], F32, 2, psum=True)
        pex = Rot(P, "pex", [128, 512], BF16, 10)
        dn = Rot(P, "dn", [128, 4], F32, 3)
        ot = Rot(P, "ot", [128, 4, 64], F32, 3)
        evs = []
        for n in range(NBLK):
            if n < NCB:
                chunks = [(c, None) for c in range(NCB)]
            else:
                m = n - NCB
                chunks = [(c, None) for c in range(NCB)]
                if m > 0:
                    chunks.append((n - 1, "L"))
                chunks.append((n, None))
                if m < NLB - 1:
                    chunks.append((n + 1, "U"))
            q_, qk = qb.next()
            P.dma("pool", q_[:], qT[:, n, :], writes=[qk])
            pes = []
            for (c, mk) in chunks:
                pS, pSk = psS.next()
                P.op("pe", lambda e, pS=pS, c=c, q_=q_: e.matmul(pS[:, :], kk[:, c * 128:(c + 1) * 128], q_[:], start=True, stop=True), reads=["kk", qk], writes=[pSk])
                pe_, pek = pex.next()
                P.op("act", lambda e, pe_=pe_, pS=pS: e.activation(out=pe_[:], in_=pS[:, :], func=AF.Exp, scale=0.125), reads=[pSk], writes=[pek])
                if mk is not None:
                    mt, mtk = (maskL, mlk) if mk == "L" else (maskU, muk)
                    P.op("dve", lambda e, pe_=pe_, mt=mt: e.tensor_tensor(pe_[:].rearrange("k (h q) -> k h q", q=128), pe_[:].rearrange("k (h q) -> k h q", q=128),
                                                                        mt[:].unsqueeze(1).broadcast_to([128, 4, 128]), ALU.mult), reads=[pek, mtk], writes=[pek])
                pes.append((pe_, pek, c))
            pO, pOk = psO.next()
            for h in range(4):
                for i_, (pe_, pek, c) in enumerate(pes):
                    P.op("pe", lambda e, pO=pO, h=h, pe_=pe_, c=c, i_=i_, nl=len(pes): e.matmul(pO[:, h * 65:(h + 1) * 65], pe_[:, h * 128:(h + 1) * 128], va[:, c, :],
                                                                                             start=(i_ == 0), stop=(i_ == nl - 1)),
                         reads=[pek, "va"], writes=[pOk])
            d_, dk = dn.next()
            pv = pO[:, 0:260].rearrange("q (h d) -> q h d", d=65)
            P.op("dve", lambda e, d_=d_, pv=pv: e.tensor_tensor(d_[:], pv[:, :, 64], es[:], ALU.add), reads=[pOk, "es"], writes=[dk])
            P.op("dve", lambda e, d_=d_: e.reciprocal(d_[:], d_[:]), reads=[dk], writes=[dk])
            o_, ok_ = ot.next()
            P.op("dve", lambda e, o_=o_, pv=pv, d_=d_: e.tensor_tensor(o_[:], pv[:, :, 0:64], d_[:].unsqueeze(2).broadcast_to([128, 4, 64]), ALU.mult), reads=[pOk, dk], writes=[ok_])
            evs.append(P.dma("sp", out[n * 128:(n + 1) * 128, :], o_[:].rearrange("q h d -> q (h d)"), reads=[ok_]))
        P.finish(evs)
        P.emit()
    return nc


def run_diff(seq, df_lam, B, T, NCX, lam_init):
    ncB = _get(("Bdf", T, NCX, lam_init), lambda: build_B_df(T, NCX, lam_init))
    lamv = _f32(np.asarray(df_lam[0]).reshape(1, 256))
    maps = []
    for cc in range(NCORE):
        qT = np.empty((2, 64, 2, T), np.float32)
        kT = np.empty((2, 64, 2, T), np.float32)
        v = np.empty((2, T, 128), np.float32)
        for lp in range(2):
            pi = cc * 2 + lp
            b, h = pi // 8, pi % 8
            qT[lp] = np.transpose(seq[b][:, h * 128:(h + 1) * 128].reshape(T, 2, 64), (2, 1, 0))
            kT[lp] = np.transpose(seq[b][:, 1024 + h * 128:1024 + (h + 1) * 128].reshape(T, 2, 64), (2, 1, 0))
            v[lp] = seq[b][:, 2048 + h * 128:2048 + (h + 1) * 128]
        maps.append({"qT": qT, "kT": kT, "v": v, "lam": lamv})
    res = _run(ncB, maps)
    o_all = np.empty((B, T, D), np.float32)
    for cc in range(NCORE):
        for lp in range(2):
            pi = cc * 2 + lp
            b, h = pi // 8, pi % 8
            o_all[b, :, h * 128:(h + 1) * 128] = res[cc]["o"][lp]
    return o_all


def run_swa(seq, sw_sinks, B, T, NCX):
    ncB = _get(("Bsw", T, NCX), lambda: build_B_sw(T, NCX))
    NBLK = T // 128
    maps = []
    for cc in range(NCORE):
        b, kv = cc // 4, cc % 4
        q = seq[b][:, kv * 256:(kv + 1) * 256].reshape(NBLK, 128, 4, 64)
        qT = _f32(np.transpose(q, (3, 0, 2, 1)).reshape(64, NBLK, 512))
        kT = _f32(seq[b][:, 1024 + kv * 64:1024 + (kv + 1) * 64].T)
        v = _f32(seq[b][:, 1280 + kv * 64:1280 + (kv + 1) * 64])
        maps.append({"qT": qT, "kT": kT, "v": v, "sinks": _f32(np.asarray(sw_sinks[0])[None, kv * 4:(kv + 1) * 4])})
    res = _run(ncB, maps)
    o_all = np.empty((B, T, D), np.float32)
    for cc in range(NCORE):
        b, kv = cc // 4, cc % 4
        o_all[b, :, kv * 256:(kv + 1) * 256] = res[cc]["o"]
    return o_all


_NC_CACHE = {}
_DBG = {}


def _get(key, fn):
    if key not in _NC_CACHE:
        _NC_CACHE[key] = fn()
    return _NC_CACHE[key]


def _run(nc, in_maps):
    res = run_bass_kernel_spmd(nc, in_maps, core_ids=list(range(NCORE)))
    return res.results


def _f32(a):
    return np.ascontiguousarray(a, dtype=np.float32)


def kernel(x, c, ctx, c_ctx, ada_w, ada_b, norm1, norm2, ml_w_in, ml_gate_b, ml_hnorm, ml_w_out,
           df_w_in, df_lam, df_hnorm, df_w_out, sw_w_in, sw_sinks, sw_w_out,
           ffn_w_in, ffn_w_out, moe_router, moe_w_in, moe_w_out, final_norm, depth=4, return_state=False):
    x = np.asarray(x, np.float32)
    xc = np.asarray(ctx, np.float32)
    B, L, _ = x.shape
    NCX = xc.shape[1]
    Ls = L // 4
    Cs = NCX // 4
    T = NCX + L
    pos_all = np.arange(L, dtype=np.float32)
    conds = [_f32(np.stack([np.asarray(c)[cc // 4], np.asarray(c_ctx)])) for cc in range(NCORE)]

    def shard_tok(lat, cx):
        outs = []
        for cc in range(NCORE):
            b, s = cc // 4, cc % 4
            outs.append(_f32(np.concatenate([lat[b, s * Ls:(s + 1) * Ls], cx[b, s * Cs:(s + 1) * Cs]], axis=0)))
        return outs

    def unshard_tok(outs, width):
        lat = np.empty((B, L, width), np.float32)
        cx = np.empty((B, NCX, width), np.float32)
        for cc in range(NCORE):
            b, s = cc // 4, cc % 4
            lat[b, s * Ls:(s + 1) * Ls] = outs[cc][:Ls]
            cx[b, s * Cs:(s + 1) * Cs] = outs[cc][Ls:]
        return lat, cx

    for i in range(depth):
        last = i == 3
        kind = ("ml", "df", "sw")[i % 3]
        jm = i // 3
        w_in = {"ml": ml_w_in, "df": df_w_in, "sw": sw_w_in}[kind][jm if kind == "ml" else 0]
        w_in = _f32(w_in)
        N = w_in.shape[1]
        xs = shard_tok(x, xc)
        aw = _f32(ada_w[i])
        ab = _f32(np.asarray(ada_b[i])[None, :])
        ncA = _get(("A", Ls, Cs, N, kind), lambda: build_A(Ls, Cs, N, kind))
        maps = []
        for cc in range(NCORE):
            m = {"x": xs[cc], "cond": conds[cc], "ada_w": aw, "ada_b": ab, "norm_g": _f32(np.asarray(norm1[i])[None, :]), "w_in": w_in}
            if kind in ("df", "sw"):
                s = cc % 4
                m["pos"] = _f32(pos_all[s * Ls:(s + 1) * Ls])
            maps.append(m)
        res = _run(ncA, maps)
        p_lat, p_ctx = unshard_tok([r["proj"] for r in res], N)
        seq = np.concatenate([p_ctx, p_lat], axis=1)
        cmaps = []
        if kind == "ml":
            GC = 5
            NCH = T // 128
            NSEG = NCH // GC
            seq_b = np.concatenate([p_ctx[:, ::-1], p_lat[:, ::-1]], axis=1)
            gbv = np.asarray(ml_gate_b[jm], np.float32)
            maps = []
            for cc in range(NCORE):
                qT = np.empty((4, 64, T), np.float32)
                kT = np.empty((4, 64, T), np.float32)
                kt = np.empty((4, T, 64), np.float32)
                vt = np.empty((4, T, 128), np.float32)
                ip = np.empty((4 * NSEG, GC * 128), np.float32)
                fp = np.empty((4 * NSEG, GC * 128), np.float32)
                gbs = np.empty((4 * NSEG, 2), np.float32)
                for lp in range(2):
                    pi = cc * 2 + lp
                    b, h = pi // 8, pi % 8
                    for d in range(2):
                        j = lp * 2 + d
                        sq = (seq, seq_b)[d][b]
                        qT[j] = sq[:, h * 64:(h + 1) * 64].T
                        kT[j] = sq[:, 512 + h * 64:512 + (h + 1) * 64].T
                        kt[j] = sq[:, 512 + h * 64:512 + (h + 1) * 64]
                        vt[j] = sq[:, 1024 + h * 128:1024 + (h + 1) * 128]
                        ip[j * NSEG:(j + 1) * NSEG] = sq[:, 3072 + (2 * d) * 8 + h].reshape(NSEG, GC * 128)
                        fp[j * NSEG:(j + 1) * NSEG] = sq[:, 3072 + (2 * d + 1) * 8 + h].reshape(NSEG, GC * 128)
                        gbs[j * NSEG:(j + 1) * NSEG, 0] = gbv[2 * d, h]
                        gbs[j * NSEG:(j + 1) * NSEG, 1] = gbv[2 * d + 1, h]
                maps.append({"qT": qT, "kT": kT, "ktok": kt, "vtok": vt, "ipre": ip, "fpre": fp, "gbias": gbs})
            ncB = _get(("Bml", T), lambda: build_B_ml(T, GC))
            res = _run(ncB, maps)
            hf = np.empty((B, T, D), np.float32)
            hb = np.empty((B, T, D), np.float32)
            for cc in range(NCORE):
                hh = res[cc]["h"]
                for lp in range(2):
                    pi = cc * 2 + lp
                    b, h = pi // 8, pi % 8
                    hf[b, :, h * 128:(h + 1) * 128] = hh[lp * 2]
                    hb[b, :NCX, h * 128:(h + 1) * 128] = hh[lp * 2 + 1][:NCX][::-1]
                    hb[b, NCX:, h * 128:(h + 1) * 128] = hh[lp * 2 + 1][NCX:][::-1]
            _DBG.update(hf=hf, hb=hb, seq=seq)
            m1s = shard_tok(hf[:, NCX:], hf[:, :NCX])
            m2s = shard_tok(hb[:, NCX:], hb[:, :NCX])
            ogs = shard_tok(p_lat[:, :, 2048:3072], p_ctx[:, :, 2048:3072])
            extra = [{"m1": m1s[cc], "m2": m2s[cc], "og": ogs[cc], "hn": _f32(np.asarray(ml_hnorm[jm])[None, :])} for cc in range(NCORE)]
            w_out = _f32(ml_w_out[jm])
            lam_init = 0.0
        elif kind == "df":
            o_all = run_diff(seq, df_lam, B, T, NCX, 0.8 - 0.6 * math.exp(-0.3 * i))
            m1s = shard_tok(o_all[:, NCX:], o_all[:, :NCX])
            extra = [{"m1": m1s[cc], "hn": _f32(np.asarray(df_hnorm[0])[None, :])} for cc in range(NCORE)]
            w_out = _f32(df_w_out[0])
            lam_init = 0.8 - 0.6 * math.exp(-0.3 * i)
        else:
            o_all = run_swa(seq, sw_sinks, B, T, NCX)
            m1s = shard_tok(o_all[:, NCX:], o_all[:, :NCX])
            extra = [{"m1": m1s[cc]} for cc in range(NCORE)]
            w_out = _f32(sw_w_out[0])
            lam_init = 0.0
        ffn = "dense" if i % 2 == 0 else "moe"
        jf = i // 2
        ncC = _get(("C", Ls, Cs, kind, ffn, last, lam_init), lambda: build_C(Ls, Cs, kind, ffn, last, lam_init))
        maps = []
        for cc in range(NCORE):
            m = {"x": xs[cc], "cond": conds[cc], "ada_w": aw, "ada_b": ab, "norm_g": _f32(np.asarray(norm2[i])[None, :]), "w_out": w_out}
            m.update(extra[cc])
            if ffn == "dense":
                m["f_in"] = _f32(np.asarray(ffn_w_in[jf])[None])
                m["f_out"] = _f32(np.asarray(ffn_w_out[jf])[None])
            else:
                m["f_in"] = _f32(moe_w_in[jf])
                m["f_out"] = _f32(moe_w_out[jf])
                m["router"] = _f32(moe_router[jf])
            if last:
                m["final_g"] = _f32(np.asarray(final_norm)[None, :])
            maps.append(m)
        res = _run(ncC, maps)
        x, xc_new = unshard_tok([r["xo"] for r in res], D)
        if not last:
            xc = xc_new
    if return_state:
        return x, xc
    return x
```

```python
import contextlib
import numpy as np
import concourse.bass as bass
import concourse.mybir as mybir

F32 = mybir.dt.float32
BF16 = mybir.dt.bfloat16
AF = mybir.ActivationFunctionType
ALU = mybir.AluOpType
AX = mybir.AxisListType

N_DMA_SEMS = 24
EPOCH_LIMIT = 28000


class Prog:
    ENGS = ("pe", "act", "dve", "pool", "sp")

    def __init__(self, nc, stack, n_epochs=6):
        self.nc = nc
        self.stack = stack
        self.eng_obj = {"pe": nc.tensor, "act": nc.scalar, "dve": nc.vector,
                        "pool": nc.gpsimd, "sp": nc.sync}
        self.lists = {e: [] for e in self.ENGS}
        self.n_epochs = n_epochs
        self.eng_sems = [{e: stack.enter_context(nc.semaphore(f"s_{e}_{k}")) for e in self.ENGS}
                         for k in range(n_epochs)]
        self.bar_sem = stack.enter_context(nc.semaphore("s_bar"))
        self.bar_count = 0
        self.dma_sems = [stack.enter_context(nc.semaphore(f"s_dma{i}")) for i in range(N_DMA_SEMS)]
        self.dma_vals = [0] * N_DMA_SEMS
        self.dma_next = 0
        self.epoch = 0
        self.cnt = {e: 0 for e in self.ENGS}
        self.state = {}
        self.waited = {e: {} for e in self.ENGS}
        self.n_ops = 0
        self.uid = 0
        self.scopes = []

    def sb(self, name, shape, dt):
        stk = self.scopes[-1] if self.scopes else self.stack
        return stk.enter_context(self.nc.sbuf_tensor(name, list(shape), dt))

    def ps(self, name, shape, dt):
        return self.stack.enter_context(self.nc.psum_tensor(name, list(shape), dt))

    def push_scope(self):
        self.scopes.append(contextlib.ExitStack())

    def pop_scope(self):
        self.barrier()
        self.scopes.pop().close()

    def _deps(self, reads, writes):
        deps = []
        for k in reads:
            st = self.state.get(k)
            if st and st[0] is not None:
                deps.append(st[0])
        for k in writes:
            st = self.state.get(k)
            if st:
                if st[0] is not None:
                    deps.append(st[0])
                deps.extend(st[1])
        return deps

    def _record(self, ev, reads, writes):
        for k in reads:
            st = self.state.setdefault(k, [None, []])
            st[1].append(ev)
        for k in writes:
            self.state[k] = [ev, []]

    def _emit_waits(self, eng, deps):
        w = self.waited[eng]
        best = {}
        for (sem, val) in deps:
            if w.get(id(sem), 0) >= val:
                continue
            if best.get(id(sem), (None, 0))[1] < val:
                best[id(sem)] = (sem, val)
        for sem, val in best.values():
            self.lists[eng].append(("wait", sem, val))
            w[id(sem)] = val

    def op(self, eng, fn, reads=(), writes=()):
        if self.cnt[eng] >= EPOCH_LIMIT:
            self.barrier()
        deps = self._deps(reads, writes)
        self._emit_waits(eng, deps)
        self.cnt[eng] += 1
        sem = self.eng_sems[self.epoch][eng]
        ev = (sem, self.cnt[eng])
        self.lists[eng].append(("op", fn, sem, 1))
        if eng == "pe":
            self.waited[eng][id(sem)] = self.cnt[eng]
        self._record(ev, reads, writes)
        self.n_ops += 1
        return ev

    def dma(self, queue, out, in_, reads=(), writes=(), **kw):
        i = self.dma_next
        self.dma_next = (self.dma_next + 1) % N_DMA_SEMS
        sem = self.dma_sems[i]
        deps = self._deps(reads, writes)
        if self.dma_vals[i] > 0:
            deps.append((sem, self.dma_vals[i]))
        self._emit_waits(queue, deps)
        self.dma_vals[i] += 16
        assert self.dma_vals[i] < 60000
        ev = (sem, self.dma_vals[i])

        def fn(e, out=out, in_=in_, kw=kw):
            return e.dma_start(out=out, in_=in_, **kw)
        self.lists[queue].append(("op", fn, sem, 16))
        self._record(ev, reads, writes)
        self.n_ops += 1
        return ev

    def barrier(self):
        live = []
        for st in self.state.values():
            if st[0] is not None:
                live.append(st[0])
            live.extend(st[1])
        dma_ids = {id(s) for s in self.dma_sems}
        live_dma = [ev for ev in live if id(ev[0]) in dma_ids]
        self._emit_waits("sp", live_dma)
        self.bar_count += 1
        n = len(self.ENGS)
        for e in self.ENGS:
            if self.cnt[e] > 0:
                self._emit_waits(e, [(self.eng_sems[self.epoch][e], self.cnt[e])])
            self.lists[e].append(("inc", self.bar_sem))
        for e in self.ENGS:
            self.lists[e].append(("wait", self.bar_sem, n * self.bar_count))
        self.epoch += 1
        assert self.epoch < self.n_epochs, "out of epochs"
        self.cnt = {e: 0 for e in self.ENGS}
        self.state = {}
        self.waited = {e: {} for e in self.ENGS}

    def finish(self, final_events):
        self._emit_waits("sp", list(final_events))

    def emit(self):
        nc = self.nc
        with nc.Block() as block:
            def mk(eng):
                def body(e):
                    for item in self.lists[eng]:
                        if item[0] == "wait":
                            e.wait_ge(item[1], item[2])
                        elif item[0] == "inc":
                            e.sem_inc(item[1], 1)
                        else:
                            ins = item[1](e)
                            ins.then_inc(item[2], item[3])
                return body
            block.tensor(mk("pe"))
            block.scalar(mk("act"))
            block.vector(mk("dve"))
            block.gpsimd(mk("pool"))
            block.sync(mk("sp"))

import math
from concourse.bass_utils import run_bass_kernel_spmd

D = 1024
SEQ = 16384
CTX = 256
NCORE = 8
I32 = mybir.dt.int32


class Rot:
    def __init__(self, P, name, shape, dt, n, psum=False):
        self.tiles = [(P.ps if psum else P.sb)(f"{name}{i}", shape, dt) for i in range(n)]
        self.keys = [f"{name}{i}" for i in range(n)]
        self.i = 0

    def next(self):
        t, k = self.tiles[self.i], self.keys[self.i]
        self.i = (self.i + 1) % len(self.tiles)
        return t, k


def dram_in(nc, name, shape, dt=F32):
    return nc.dram_tensor(name, list(shape), dt, kind="ExternalInput").ap()


def dram_out(nc, name, shape, dt=F32):
    return nc.dram_tensor(name, list(shape), dt, kind="ExternalOutput").ap()


def make_ident(P, name, dt, n=128):
    f = P.sb(name + "_f", [128, 128], F32)
    P.op("pool", lambda e: e.memset(f[:], 0.0), writes=[name + "_f"])
    P.op("pool", lambda e: e.affine_select(out=f[:], in_=f[:], pattern=[[-1, 128]], compare_op=ALU.not_equal,
                                           fill=1.0, base=0, channel_multiplier=1),
         reads=[name + "_f"], writes=[name + "_f"])
    if dt == F32:
        return f, name + "_f"
    b = P.sb(name, [128, 128], dt)
    P.op("dve", lambda e: e.tensor_copy(b[:], f[:]), reads=[name + "_f"], writes=[name])
    return b, name


def make_tri(P, name, upper, dt=F32):
    f = P.sb(name, [128, 128], dt)
    P.op("pool", lambda e: e.memset(f[:], 1.0), writes=[name])
    if upper:
        P.op("pool", lambda e: e.affine_select(out=f[:], in_=f[:], pattern=[[1, 128]], compare_op=ALU.is_ge,
                                               fill=0.0, base=0, channel_multiplier=-1), reads=[name], writes=[name])
    else:
        P.op("pool", lambda e: e.affine_select(out=f[:], in_=f[:], pattern=[[-1, 128]], compare_op=ALU.is_ge,
                                               fill=0.0, base=0, channel_multiplier=1), reads=[name], writes=[name])
    return f, name


def view(t, off, shape):
    n = 1
    for d_ in shape[1:]:
        n *= d_
    ap = t[:, off:off + n]
    if len(shape) == 3:
        ap = ap.rearrange("p (a b) -> p a b", b=shape[2])
    elif len(shape) == 4:
        ap = ap.rearrange("p (a b c) -> p a b c", b=shape[2], c=shape[3])
    return ap


class VRot:
    def __init__(self, name, views):
        self.tiles = views
        self.keys = [f"{name}{i}" for i in range(len(views))]
        self.i = 0

    def next(self):
        t, k = self.tiles[self.i], self.keys[self.i]
        self.i = (self.i + 1) % len(self.tiles)
        return t, k


def emit_mod(P, cond, ada_w, ada_b, chunks, ps_rot, tag, res, scr):
    cp = P.sb(tag + "cp", [128, 2, 8], F32)
    cs = P.sb(tag + "cs", [128, 2, 8], F32)
    ones = P.sb(tag + "ones", [128, 128], F32)
    cB = view(scr, 4096, [128, 2, 8, 128])
    P.dma("sp", cp[:], cond.rearrange("c (k p) -> p c k", p=128), writes=[tag + "cp"], allow_slow_non_contiguous=True)
    P.op("act", lambda e: e.activation(out=cs[:], in_=cp[:], func=AF.Silu), reads=[tag + "cp"], writes=[tag + "cs"])
    P.op("dve", lambda e: e.memset(ones[:], 1.0), writes=[tag + "ones"])
    for ci in range(2):
        for k in range(8):
            P.op("dve", lambda e, ci=ci, k=k: e.tensor_scalar(cB[:, ci, k, :], ones[:], cs[:, ci, k:k + 1], None, ALU.mult),
                 reads=[tag + "ones", tag + "cs"], writes=[tag + "cB"])
    MW = 256
    wrot = VRot(tag + "aw", [view(scr, i * 2048, [128, 8, MW]) for i in range(2)])
    brot = VRot(tag + "ab", [view(scr, 6144 + i * MW, [128, MW]) for i in range(2)])
    for j in chunks:
        for n in range(1024 // MW):
            c0 = j * 1024 + n * MW
            wt, wk = wrot.next()
            bt, bk = brot.next()
            P.dma("sp", wt, ada_w[:, c0:c0 + MW].rearrange("(k p) n -> p k n", p=128), writes=[wk])
            P.dma("sp", bt, ada_b[0:1, c0:c0 + MW].partition_broadcast(128), writes=[bk])
            for ci in range(2):
                pt, pk = ps_rot.next()
                for k in range(8):
                    P.op("pe", lambda e, pt=pt, wt=wt, ci=ci, k=k: e.matmul(pt[:, 0:MW], cB[:, ci, k, :], wt[:, k, :],
                                                                          start=(k == 0), stop=(k == 7)),
                         reads=[tag + "cB", wk], writes=[pk])
                t, tk = res[(ci, j)]
                P.op("dve", lambda e, t=t, pt=pt, bt=bt, n=n: e.tensor_tensor(t[:, n * MW:(n + 1) * MW], pt[:, 0:MW], bt, ALU.add),
                     reads=[pk, bk], writes=[tk])
    return res


def fold_gain(P, norm_g, mods, j_scale, tag):
    gb = P.sb(tag + "gb", [128, 1024], F32)
    P.dma("sp", gb[:], norm_g[0:1, :].partition_broadcast(128), writes=[tag + "gb"])
    for ci in range(2):
        t, tk = mods[(ci, j_scale)]
        P.op("dve", lambda e, t=t: e.scalar_tensor_tensor(t[:], t[:], 1.0, gb[:], ALU.add, ALU.mult),
             reads=[tk, tag + "gb"], writes=[tk])


class NormCtx:
    def __init__(self, P, tag, ident_bf, ident_key):
        self.P = P
        self.tag = tag
        self.sq = Rot(P, tag + "sq", [128, 1024], F32, 1)
        self.ss = Rot(P, tag + "ss", [128, 1], F32, 2)
        self.rs = Rot(P, tag + "rs", [128, 1], F32, 2)
        self.tmp = Rot(P, tag + "tmp", [128, 1024], F32, 1)
        self.hb = Rot(P, tag + "hb", [128, 1024], BF16, 2)
        self.ident, self.ik = ident_bf, ident_key

    def norm_mod(self, x, xk, rows, G, Gk, S, Sk, out_f32=None):
        P = self.P
        sq, sqk = self.sq.next()
        ss, ssk = self.ss.next()
        rs, rsk = self.rs.next()
        P.op("act", lambda e: e.activation(out=sq[:rows], in_=x[:rows], func=AF.Square, accum_out=ss[:rows]),
             reads=[xk], writes=[sqk, ssk])
        P.op("dve", lambda e: e.tensor_scalar(rs[:rows], ss[:rows], 1.0 / D, 1e-6, ALU.mult, ALU.add), reads=[ssk], writes=[rsk])
        P.op("act", lambda e: e.activation(out=rs[:rows], in_=rs[:rows], func=AF.Sqrt), reads=[rsk], writes=[rsk])
        P.op("dve", lambda e: e.reciprocal(rs[:rows], rs[:rows]), reads=[rsk], writes=[rsk])
        tmp, tk = self.tmp.next()
        P.op("dve", lambda e: e.scalar_tensor_tensor(tmp[:rows], x[:rows], rs[:rows, 0:1], G[:rows], ALU.mult, ALU.mult),
             reads=[xk, rsk, Gk], writes=[tk])
        hb, hk = self.hb.next()
        if out_f32 is not None:
            of, ofk = out_f32
            P.op("pool", lambda e: e.tensor_tensor(of[:rows], tmp[:rows], S[:rows], ALU.add), reads=[tk, Sk], writes=[ofk])
            P.op("act", lambda e: e.copy(hb[:rows], of[:rows]), reads=[ofk], writes=[hk])
        else:
            P.op("pool", lambda e: e.tensor_tensor(hb[:rows], tmp[:rows], S[:rows], ALU.add), reads=[tk, Sk], writes=[hk])
        return hb, hk


def transpose_to(P, src, srck, rows, dst_fn, dstk, ps_rot, ident, ik, dt=BF16, evac="act"):
    pt, pk = ps_rot.next()
    pv = pt[:].bitcast(dt) if dt != F32 else pt[:]
    nper = 8 if dt != F32 else 4
    for half in range(8 // nper):
        if half > 0:
            pt, pk = ps_rot.next()
            pv = pt[:]
        for kk in range(nper):
            k = half * nper + kk
            P.op("pe", lambda e, pv=pv, kk=kk, k=k: e.transpose(pv[:, kk * 128:kk * 128 + rows], src[:rows, k * 128:(k + 1) * 128], ident[:rows, :rows]),
                 reads=[srck, ik], writes=[pk])
        for kk in range(nper):
            k = half * nper + kk
            if evac == "act":
                P.op("act", lambda e, pv=pv, kk=kk, k=k: e.copy(dst_fn(k), pv[:, kk * 128:kk * 128 + rows]), reads=[pk], writes=[dstk])
            else:
                P.op("dve", lambda e, pv=pv, kk=kk, k=k: e.tensor_copy(dst_fn(k), pv[:, kk * 128:kk * 128 + rows]), reads=[pk], writes=[dstk])


def token_tiles(n_lat, n_ctx):
    tiles = []
    r = 0
    while r < n_lat:
        tiles.append((r, 128, 0))
        r += 128
    r = 0
    while r < n_ctx:
        rows = min(128, n_ctx - r)
        tiles.append((n_lat + r, rows, 1))
        r += rows
    return tiles


def load_w_bf16(P, dst, dstk, w, ncols, col0=0, kchunks=8, queue="pool"):
    for k in range(kchunks):
        P.dma(queue, dst[:, k, 0:ncols], w[k * 128:(k + 1) * 128, col0:col0 + ncols], writes=[dstk])


def build_A(n_lat, n_ctx, N, kind):
    nc = bass.Bass("TRN2", target_bir_lowering=False)
    NT = n_lat + n_ctx
    x = dram_in(nc, "x", [NT, D])
    cond = dram_in(nc, "cond", [2, D])
    ada_w = dram_in(nc, "ada_w", [D, 6 * D])
    ada_b = dram_in(nc, "ada_b", [1, 6 * D])
    ng = dram_in(nc, "norm_g", [1, D])
    w = dram_in(nc, "w_in", [D, N])
    rope = kind in ("df", "sw")
    if rope:
        pos = dram_in(nc, "pos", [n_lat])
    out = dram_out(nc, "proj", [NT, N])
    nh_rope = {"df": 32, "sw": 20}.get(kind, 0)
    with contextlib.ExitStack() as st:
        P = Prog(nc, st, n_epochs=4)
        psr = Rot(P, "ps", [128, 512], F32, 6, psum=True)
        ident, ik = make_ident(P, "ident", BF16)
        mod_tiles = {(ci, j): (P.sb(f"mmod{ci}_{j}", [128, 1024], F32), f"mmod{ci}_{j}") for j in [0, 1] for ci in range(2)}
        scr = P.sb("scr", [128, max(2 * N, 6656)], F32)
        mods = emit_mod(P, cond, ada_w, ada_b, [0, 1], psr, "m", mod_tiles, scr)
        fold_gain(P, ng, mods, 1, "m")
        P.barrier()
        W = P.sb("W", [128, 8, N], BF16)
        load_w_bf16(P, W, "W", w, N)
        ntl = n_lat // 128
        if rope:
            post = P.sb("post", [128, ntl], F32)
            P.dma("sp", post[:], pos.rearrange("(t p) -> p t", p=128), writes=["post"], allow_slow_non_contiguous=True)
            colt = P.sb("colt", [128, ntl], F32)
            rowt = P.sb("rowt", [128, ntl], F32)
            ti = P.sb("rti", [128, ntl], I32)
            P.op("dve", lambda e: e.tensor_scalar(rowt[:], post[:], 1.0 / 64, None, ALU.mult), reads=["post"], writes=["rowt"])
            P.op("dve", lambda e: e.tensor_copy(ti[:], rowt[:]), reads=["rowt"], writes=["rti"])
            P.op("dve", lambda e: e.tensor_copy(colt[:], ti[:]), reads=["rti"], writes=["colt"])
            gt = P.sb("rgt", [128, ntl], F32)
            P.op("dve", lambda e: e.tensor_tensor(gt[:], colt[:], rowt[:], ALU.is_gt), reads=["colt", "rowt"], writes=["rgt"])
            P.op("dve", lambda e: e.tensor_sub(rowt[:], colt[:], gt[:]), reads=["colt", "rgt", "rowt"], writes=["rowt"])
            P.op("dve", lambda e: e.scalar_tensor_tensor(colt[:], rowt[:], -64.0, post[:], ALU.mult, ALU.add), reads=["rowt", "post", "colt"], writes=["colt"])
            inv = P.sb("inv", [128, 16], F32)
            P.op("pool", lambda e: e.iota(inv[:], [[1, 16]], base=0, channel_multiplier=0, allow_small_or_imprecise_dtypes=True), writes=["inv"])
            P.op("act", lambda e: e.activation(out=inv[:], in_=inv[:], func=AF.Exp, scale=-math.log(10000.0) / 16), reads=["inv"], writes=["inv"])
            ang = view(scr, 0, [128, ntl, 32])
            P.op("dve", lambda e: e.tensor_tensor(ang[:, :, 0:16], rowt[:].unsqueeze(2).broadcast_to([128, ntl, 16]),
                                                  inv[:].unsqueeze(1).broadcast_to([128, ntl, 16]), ALU.mult), reads=["rowt", "inv"], writes=["ang"])
            P.op("dve", lambda e: e.tensor_tensor(ang[:, :, 16:32], colt[:].unsqueeze(2).broadcast_to([128, ntl, 16]),
                                                  inv[:].unsqueeze(1).broadcast_to([128, ntl, 16]), ALU.mult), reads=["colt", "inv"], writes=["ang"])
            sint = P.sb("sint", [128, ntl, 32], F32)
            cost = P.sb("cost", [128, ntl, 32], F32)
            rf = view(scr, ntl * 32, [128, ntl, 32])
            ri = view(scr, 2 * ntl * 32, [128, ntl, 32]).bitcast(I32)
            rm = view(scr, 3 * ntl * 32, [128, ntl, 32])
            for (dst, dk, off) in ((sint, "sint", 0.0), (cost, "cost", 0.25)):
                P.op("dve", lambda e, dst=dst, off=off: e.tensor_scalar(dst[:], ang, 1.0 / (2 * math.pi), off, ALU.mult, ALU.add), reads=["ang"], writes=[dk])
                P.op("dve", lambda e, dst=dst: e.tensor_copy(ri, dst[:]), reads=[dk], writes=["ri"])
                P.op("dve", lambda e: e.tensor_copy(rf, ri), reads=["ri"], writes=["rf"])
                P.op("dve", lambda e, dst=dst: e.tensor_sub(dst[:], dst[:], rf), reads=[dk, "rf"], writes=[dk])
                P.op("dve", lambda e, dst=dst: e.tensor_scalar(rm, dst[:], 0.5, None, ALU.is_gt), reads=[dk], writes=["rm"])
                P.op("dve", lambda e, dst=dst: e.tensor_sub(dst[:], dst[:], rm), reads=[dk, "rm"], writes=[dk])
                P.op("dve", lambda e, dst=dst: e.tensor_scalar(rm, dst[:], -0.5, None, ALU.is_lt), reads=[dk], writes=["rm"])
                P.op("dve", lambda e, dst=dst: e.tensor_add(dst[:], dst[:], rm), reads=[dk, "rm"], writes=[dk])
                P.op("act", lambda e, dst=dst: e.activation(out=dst[:], in_=dst[:], func=AF.Sin, scale=2 * math.pi), reads=[dk], writes=[dk])
            rt = [P.sb(f"rt{i}", [128, nh_rope, 32], F32) for i in range(4)]
            P.barrier()
        nrm = NormCtx(P, "n", ident, ik)
        xr = Rot(P, "x", [128, D], F32, 2)
        hT = Rot(P, "hT", [128, 8, 128], BF16, 2)
        ot = VRot("ot", [view(scr, i * N, [128, N]) for i in range(2)])
        evs = []
        nch = (N + 511) // 512
        for (r0, rows, ci) in token_tiles(n_lat, n_ctx):
            xt, xk = xr.next()
            P.dma("sp", xt[:rows], x[r0:r0 + rows, :], writes=[xk])
            G, Gk = mods[(ci, 1)]
            S, Sk = mods[(ci, 0)]
            hb, hk = nrm.norm_mod(xt, xk, rows, G, Gk, S, Sk)
            ht, htk = hT.next()
            transpose_to(P, hb, hk, rows, lambda k, ht=ht, rows=rows: ht[:, k, :rows], htk, psr, ident, ik)
            o, okk = ot.next()
            for n in range(nch):
                c0 = n * 512
                cw = min(512, N - c0)
                pt, pk = psr.next()
                for k in range(8):
                    P.op("pe", lambda e, pt=pt, ht=ht, k=k, c0=c0, cw=cw, rows=rows: e.matmul(pt[:rows, 0:cw], ht[:, k, :rows], W[:, k, c0:c0 + cw],
                                                                                   start=(k == 0), stop=(k == 7)),
                         reads=[htk, "W"], writes=[pk])
                if kind == "ml" and n == 1:
                    P.op("act", lambda e, pt=pt, o=o, c0=c0, cw=cw, rows=rows: e.mul(o[:rows, c0:c0 + cw], pt[:rows, 0:cw], ML_KSCALE), reads=[pk], writes=[okk])
                elif n % 2 == 0:
                    P.op("dve", lambda e, pt=pt, o=o, c0=c0, cw=cw, rows=rows: e.tensor_copy(o[:rows, c0:c0 + cw], pt[:rows, 0:cw]), reads=[pk], writes=[okk])
                else:
                    P.op("act", lambda e, pt=pt, o=o, c0=c0, cw=cw, rows=rows: e.copy(o[:rows, c0:c0 + cw], pt[:rows, 0:cw]), reads=[pk], writes=[okk])
            if rope and ci == 0:
                tix = r0 // 128
                ov = o[:, 0:nh_rope * 64].rearrange("p (h two j) -> p h two j", two=2, j=32)
                x1 = ov[:, :, 0, :]
                x2 = ov[:, :, 1, :]
                cb = cost[:, tix, :].unsqueeze(1).broadcast_to([128, nh_rope, 32])
                sb_ = sint[:, tix, :].unsqueeze(1).broadcast_to([128, nh_rope, 32])
                P.op("pool", lambda e, x1=x1, cb=cb: e.tensor_tensor(rt[0][:], x1, cb, ALU.mult), reads=[okk, "cost"], writes=["rt0"])
                P.op("dve", lambda e, x2=x2, sb_=sb_: e.tensor_tensor(rt[1][:], x2, sb_, ALU.mult), reads=[okk, "sint"], writes=["rt1"])
                P.op("pool", lambda e, x1=x1, sb_=sb_: e.tensor_tensor(rt[2][:], x1, sb_, ALU.mult), reads=[okk, "sint"], writes=["rt2"])
                P.op("dve", lambda e, x2=x2, cb=cb: e.tensor_tensor(rt[3][:], x2, cb, ALU.mult), reads=[okk, "cost"], writes=["rt3"])
                P.op("pool", lambda e, x1=x1: e.tensor_tensor(x1, rt[0][:], rt[1][:], ALU.subtract), reads=["rt0", "rt1", okk], writes=[okk])
                P.op("dve", lambda e, x2=x2: e.tensor_tensor(x2, rt[2][:], rt[3][:], ALU.add), reads=["rt2", "rt3", okk], writes=[okk])
            evs.append(P.dma("act", out[r0:r0 + rows, :], o[:rows, :], reads=[okk]))
        P.finish(evs)
        P.emit()
    return nc


ML_KSCALE = 64 ** -0.5


def build_C(n_lat, n_ctx, kind, ffn, last, lam_init=0.0):
    nc = bass.Bass("TRN2", target_bir_lowering=False)
    NT = n_lat + n_ctx
    x = dram_in(nc, "x", [NT, D])
    m1 = dram_in(nc, "m1", [NT, D])
    if kind == "ml":
        m2 = dram_in(nc, "m2", [NT, D])
        og = dram_in(nc, "og", [NT, D])
    if kind in ("ml", "df"):
        hn = dram_in(nc, "hn", [1, D])
    cond = dram_in(nc, "cond", [2, D])
    ada_w = dram_in(nc, "ada_w", [D, 6 * D])
    ada_b = dram_in(nc, "ada_b", [1, 6 * D])
    ng = dram_in(nc, "norm_g", [1, D])
    wo = dram_in(nc, "w_out", [D, D])
    if ffn == "dense":
        F = 2816
        NE = 1
        f_in = dram_in(nc, "f_in", [1, D, 2 * F])
        f_out = dram_in(nc, "f_out", [1, F, D])
    else:
        F = 3584
        NE = 8
        f_in = dram_in(nc, "f_in", [8, D, 2 * F])
        f_out = dram_in(nc, "f_out", [8, F, D])
        router = dram_in(nc, "router", [D, 8])
    if last:
        fg = dram_in(nc, "final_g", [1, D])
    out = dram_out(nc, "xo", [NT, D])
    f_in_bf = nc.dram_tensor("f_in_bf", [NE, D, 2 * F], BF16).ap()
    f_out_bf = nc.dram_tensor("f_out_bf", [NE, F, D], BF16).ap()
    NFC = F // 128
    FB = 2
    with contextlib.ExitStack() as st:
        P = Prog(nc, st, n_epochs=10)
        psA = Rot(P, "psA", [128, 512], F32, 4, psum=True)
        psB = Rot(P, "psB", [128, 512], F32, 3, psum=True)
        psT = Rot(P, "psT", [128, 512], F32, 1, psum=True)
        ident, ik = make_ident(P, "ident", BF16)
        mod_tiles = {(ci, j): (P.sb(f"mmod{ci}_{j}", [128, 1024], F32), f"mmod{ci}_{j}") for j in [2, 3, 4, 5] for ci in range(2)}
        scr = P.sb("scr", [128, 8192], F32)
        mods = emit_mod(P, cond, ada_w, ada_b, [2, 3, 4, 5], psA, "m", mod_tiles, scr)
        fold_gain(P, ng, mods, 4, "m")
        P.barrier()
        scr_bf = scr[:].bitcast(BF16)
        stg = VRot("stg", [scr_bf[:, i * 8192:i * 8192 + 8192] for i in range(2)])
        NJ_O = NFC // 4
        for ex in range(NE):
            for k in range(8):
                sg, sgk = stg.next()
                P.dma("pool", sg[:, 0:2 * F], f_in[ex, k * 128:(k + 1) * 128, :], writes=[sgk])
                P.dma("sp", f_in_bf[ex, k * 128:(k + 1) * 128, :], sg[:, 0:2 * F], reads=[sgk], writes=[f"finbf{ex}_{k}"])
            for q in range(0, NFC, NJ_O):
                nj = min(NJ_O, NFC - q)
                sg, sgk = stg.next()
                sv = sg[:, 0:nj * D].rearrange("p (j n) -> p j n", n=D)
                P.dma("pool", sv, f_out[ex, q * 128:(q + nj) * 128, :].rearrange("(j p) n -> p j n", p=128), writes=[sgk])
                P.dma("sp", f_out_bf[ex, q * 128:(q + nj) * 128, :].rearrange("(j p) n -> p j n", p=128), sv, reads=[sgk], writes=[f"foutbf{ex}_{q}"])
        P.barrier()
        Wo = P.sb("Wo", [128, 8, D], BF16)
        load_w_bf16(P, Wo, "Wo", wo, D)
        if kind in ("ml", "df"):
            hnb = P.sb("hnb", [128, D], F32)
            P.dma("sp", hnb[:], hn[0:1, :].partition_broadcast(128), writes=["hnb"])
            if kind == "df":
                P.op("dve", lambda e: e.tensor_scalar(hnb[:], hnb[:], 1.0 - lam_init, None, ALU.mult), reads=["hnb"], writes=["hnb"])
        if last:
            fgb = P.sb("fgb", [128, D], F32)
            P.dma("sp", fgb[:], fg[0:1, :].partition_broadcast(128), writes=["fgb"])
        if ffn == "moe":
            identf, ifk = make_ident(P, "identf", F32)
            Rt = P.sb("Rt", [128, 8, 8], F32)
            P.dma("sp", Rt[:], router.rearrange("(k p) e -> p k e", p=128), writes=["Rt"])
            h2f = Rot(P, "h2f", [128, D], F32, 1)
            h2Tf = Rot(P, "h2Tf", [128, 8, 128], F32, 1)
            gates = P.sb("gates", [128, 4, 8], F32)
        acc = view(scr, 4096, [128, 4, D])
        nrm = NormCtx(P, "n", ident, ik)
        xr = Rot(P, "x", [128, D], F32, 1)
        ar = Rot(P, "ma", [128, D], F32, 2)
        br = Rot(P, "mb", [128, D], F32, 2) if kind == "ml" else None
        yb = Rot(P, "yb", [128, D], BF16, 2)
        yT = Rot(P, "yT", [128, 8, 128], BF16, 2)
        x1 = view(scr, 0, [128, 4, D])
        h2T = P.sb("h2T", [128, 8, 512], BF16)
        hid = P.sb("hid", [128, 2 * FB, 512], BF16)
        wina = Rot(P, "wina", [128, 8, FB * 128], BF16, 2)
        winb = Rot(P, "winb", [128, 8, FB * 128], BF16, 2)
        wout = Rot(P, "wout", [128, FB, D], BF16, 2)
        sil = Rot(P, "sil", [128, 512], F32, 2)
        st8 = Rot(P, "st8", [128, 8], F32, 4)
        st1 = Rot(P, "st1", [128, 1], F32, 6)
        xo = Rot(P, "xo", [128, D], F32, 2)
        evs = []
        tiles = token_tiles(n_lat, n_ctx)
        groups = []
        lat_tiles = [t for t in tiles if t[2] == 0]
        for g0 in range(0, len(lat_tiles), 4):
            groups.append(lat_tiles[g0:g0 + 4])
        ctx_tiles = [t for t in tiles if t[2] == 1]
        if ctx_tiles:
            groups.append(ctx_tiles)
        for grp in groups:
            gw = sum(t[1] for t in grp)
            offs = []
            o_ = 0
            for t in grp:
                offs.append(o_)
                o_ += t[1]
            for ti, (r0, rows, ci) in enumerate(grp):
                xt, xk = xr.next()
                P.dma("sp", xt[:rows], x[r0:r0 + rows, :], writes=[xk])
                a, ak = ar.next()
                P.dma("sp", a[:rows], m1[r0:r0 + rows, :], writes=[ak])
                if kind == "ml":
                    b, bk = br.next()
                    P.dma("sp", b[:rows], m2[r0:r0 + rows, :], writes=[bk])
                    P.op("pool", lambda e, a=a, b=b, rows=rows: e.tensor_tensor(a[:rows], a[:rows], b[:rows], ALU.add), reads=[ak, bk], writes=[ak])
                    b, bk = br.next()
                    P.dma("sp", b[:rows], og[r0:r0 + rows, :], writes=[bk])
                    P.op("act", lambda e, b=b, rows=rows: e.activation(out=b[:rows], in_=b[:rows], func=AF.Sigmoid), reads=[bk], writes=[bk])
                y, yk = yb.next()
                if kind in ("ml", "df"):
                    sq, sqk = nrm.sq.next()
                    P.op("act", lambda e, sq=sq, a=a, rows=rows: e.activation(out=sq[:rows], in_=a[:rows], func=AF.Square), reads=[ak], writes=[sqk])
                    s8, s8k = st8.next()
                    P.op("dve", lambda e, s8=s8, sq=sq, rows=rows: e.tensor_reduce(s8[:rows], sq[:rows].rearrange("p (h v) -> p h v", v=128), AX.X, ALU.add),
                         reads=[sqk], writes=[s8k])
                    P.op("dve", lambda e, s8=s8, rows=rows: e.tensor_scalar(s8[:rows], s8[:rows], 1.0 / 128, 1e-6, ALU.mult, ALU.add), reads=[s8k], writes=[s8k])
                    P.op("act", lambda e, s8=s8, rows=rows: e.activation(out=s8[:rows], in_=s8[:rows], func=AF.Sqrt), reads=[s8k], writes=[s8k])
                    P.op("dve", lambda e, s8=s8, rows=rows: e.reciprocal(s8[:rows], s8[:rows]), reads=[s8k], writes=[s8k])
                    av = a[:rows].rearrange("p (h v) -> p h v", v=128)
                    P.op("dve", lambda e, av=av, s8=s8, rows=rows: e.tensor_tensor(av, av, s8[:rows].unsqueeze(2).broadcast_to([rows, 8, 128]), ALU.mult),
                         reads=[ak, s8k], writes=[ak])
                    if kind == "ml":
                        P.op("pool", lambda e, a=a, rows=rows: e.tensor_tensor(a[:rows], a[:rows], hnb[:rows], ALU.mult), reads=[ak, "hnb"], writes=[ak])
                        P.op("dve", lambda e, y=y, a=a, b=b, rows=rows: e.tensor_tensor(y[:rows], a[:rows], b[:rows], ALU.mult), reads=[ak, bk], writes=[yk])
                    else:
                        P.op("pool", lambda e, y=y, a=a, rows=rows: e.tensor_tensor(y[:rows], a[:rows], hnb[:rows], ALU.mult), reads=[ak, "hnb"], writes=[yk])
                else:
                    P.op("act", lambda e, y=y, a=a, rows=rows: e.copy(y[:rows], a[:rows]), reads=[ak], writes=[yk])
                yt, ytk = yT.next()
                transpose_to(P, y, yk, rows, lambda k, yt=yt, rows=rows: yt[:, k, :rows], ytk, psT, ident, ik)
                gm, gmk = mods[(ci, 2)]
                for n in range(2):
                    pt, pk = psB.next()
                    for k in range(8):
                        P.op("pe", lambda e, pt=pt, yt=yt, k=k, n=n, rows=rows: e.matmul(pt[:rows, :], yt[:, k, :rows], Wo[:, k, n * 512:(n + 1) * 512],
                                                                                         start=(k == 0), stop=(k == 7)),
                             reads=[ytk, "Wo"], writes=[pk])
                    P.op("dve", lambda e, pt=pt, n=n, ti=ti, rows=rows, gm=gm: e.tensor_tensor(x1[:rows, ti, n * 512:(n + 1) * 512], pt[:rows, :], gm[:rows, n * 512:(n + 1) * 512], ALU.mult),
                         reads=[pk, gmk], writes=[f"x1_{ti}"])
                P.op("pool", lambda e, ti=ti, rows=rows, xt=xt: e.tensor_tensor(x1[:rows, ti, :], x1[:rows, ti, :], xt[:rows], ALU.add), reads=[f"x1_{ti}", xk], writes=[f"x1_{ti}"])
                G, Gk = mods[(ci, 4)]
                S, Sk = mods[(ci, 3)]
                if ffn == "moe":
                    hf, hfk = h2f.next()
                    hb, hk = nrm.norm_mod(x1[:, ti, :], f"x1_{ti}", rows, G, Gk, S, Sk, out_f32=(hf, hfk))
                else:
                    hb, hk = nrm.norm_mod(x1[:, ti, :], f"x1_{ti}", rows, G, Gk, S, Sk)
                o0 = offs[ti]
                transpose_to(P, hb, hk, rows, lambda k, o0=o0, rows=rows: h2T[:, k, o0:o0 + rows], "h2T", psT, ident, ik)
                if ffn == "moe":
                    htf, htfk = h2Tf.next()
                    transpose_to(P, hf, hfk, rows, lambda k, htf=htf, rows=rows: htf[:, k, :rows], htfk, psT, identf, ifk, dt=F32, evac="dve")
                    pt, pk = psB.next()
                    for k in range(8):
                        P.op("pe", lambda e, pt=pt, htf=htf, k=k, rows=rows: e.matmul(pt[:rows, 0:8], htf[:, k, :rows], Rt[:, k, :], start=(k == 0), stop=(k == 7)),
                             reads=[htfk, "Rt"], writes=[pk])
                    lg, lgk = st8.next()
                    P.op("dve", lambda e, lg=lg, pt=pt, rows=rows: e.tensor_copy(lg[:rows], pt[:rows, 0:8]), reads=[pk], writes=[lgk])
                    mx1, mx1k = st1.next()
                    P.op("dve", lambda e, mx1=mx1, lg=lg, rows=rows: e.tensor_reduce(mx1[:rows], lg[:rows], AX.X, ALU.max), reads=[lgk], writes=[mx1k])
                    eq, eqk = st8.next()
                    P.op("dve", lambda e, eq=eq, lg=lg, mx1=mx1, rows=rows: e.tensor_scalar(eq[:rows], lg[:rows], mx1[:rows, 0:1], None, ALU.is_equal), reads=[lgk, mx1k], writes=[eqk])
                    P.op("dve", lambda e, eq=eq, lg=lg, rows=rows: e.scalar_tensor_tensor(eq[:rows], eq[:rows], -1e30, lg[:rows], ALU.mult, ALU.add), reads=[eqk, lgk], writes=[eqk])
                    mx2, mx2k = st1.next()
                    P.op("dve", lambda e, mx2=mx2, eq=eq, rows=rows: e.tensor_reduce(mx2[:rows], eq[:rows], AX.X, ALU.max), reads=[eqk], writes=[mx2k])
                    P.op("dve", lambda e, eq=eq, lg=lg, mx2=mx2, rows=rows: e.tensor_scalar(eq[:rows], lg[:rows], mx2[:rows, 0:1], None, ALU.is_ge), reads=[lgk, mx2k, eqk], writes=[eqk])
                    nm, nmk = st1.next()
                    P.op("dve", lambda e, nm=nm, mx1=mx1, rows=rows: e.tensor_scalar(nm[:rows], mx1[:rows], -1.0, None, ALU.mult), reads=[mx1k], writes=[nmk])
                    P.op("act", lambda e, lg=lg, nm=nm, rows=rows: e.activation(out=lg[:rows], in_=lg[:rows], func=AF.Exp, bias=nm[:rows, 0:1]), reads=[lgk, nmk], writes=[lgk])
                    P.op("dve", lambda e, lg=lg, eq=eq, rows=rows: e.tensor_tensor(lg[:rows], lg[:rows], eq[:rows], ALU.mult), reads=[lgk, eqk], writes=[lgk])
                    P.op("dve", lambda e, mx2=mx2, lg=lg, rows=rows: e.tensor_reduce(mx2[:rows], lg[:rows], AX.X, ALU.add), reads=[lgk, mx2k], writes=[mx2k])
                    P.op("dve", lambda e, mx2=mx2, rows=rows: e.reciprocal(mx2[:rows], mx2[:rows]), reads=[mx2k], writes=[mx2k])
                    P.op("dve", lambda e, lg=lg, mx2=mx2, ti=ti, rows=rows: e.tensor_scalar(gates[:rows, ti, :], lg[:rows], mx2[:rows, 0:1], None, ALU.mult), reads=[lgk, mx2k], writes=[f"gates{ti}"])
            pending = None
            blocks = [(ex, fb) for ex in range(NE) for fb in range(NFC // FB)]
            for (ex, fb) in blocks:
                if True:
                    wa, wak = wina.next()
                    wb, wbk = winb.next()
                    wo2, wo2k = wout.next()
                    f0 = fb * FB * 128
                    fin_v = f_in_bf[ex].rearrange("(k p) n -> p k n", p=128)
                    P.dma("sp", wa[:], fin_v[:, :, f0:f0 + FB * 128], writes=[wak])
                    P.dma("sp", wb[:], fin_v[:, :, F + f0:F + f0 + FB * 128], writes=[wbk])
                    P.dma("sp", wo2[:], f_out_bf[ex, f0:f0 + FB * 128, :].rearrange("(j p) n -> p j n", p=128), writes=[wo2k])
                    for j in range(FB):
                        fc = (fb % 2) * FB + j
                        pa, pak = psA.next()
                        pb, pbk = psA.next()
                        for k in range(8):
                            P.op("pe", lambda e, pa=pa, wa=wa, k=k, j=j, gw=gw: e.matmul(pa[:, 0:gw], wa[:, k, j * 128:(j + 1) * 128], h2T[:, k, 0:gw], start=(k == 0), stop=(k == 7)),
                                 reads=[wak, "h2T"], writes=[pak])
                        for k in range(8):
                            P.op("pe", lambda e, pb=pb, wb=wb, k=k, j=j, gw=gw: e.matmul(pb[:, 0:gw], wb[:, k, j * 128:(j + 1) * 128], h2T[:, k, 0:gw], start=(k == 0), stop=(k == 7)),
                                 reads=[wbk, "h2T"], writes=[pbk])
                        sl, slk = sil.next()
                        P.op("act", lambda e, sl=sl, pa=pa, gw=gw: e.activation(out=sl[:, 0:gw], in_=pa[:, 0:gw], func=AF.Silu), reads=[pak], writes=[slk])
                        P.op("dve", lambda e, sl=sl, pb=pb, fc=fc, gw=gw: e.tensor_tensor(hid[:, fc, 0:gw], sl[:, 0:gw], pb[:, 0:gw], ALU.mult), reads=[slk, pbk], writes=[f"hid{fc}"])

                    def second_stage(ex=ex, fb=fb, wo2=wo2, wo2k=wo2k):
                        for ti, (r0, rows, ci) in enumerate(grp):
                            o0 = offs[ti]
                            for n in range(2):
                                pt, pk = psB.next()
                                for j in range(FB):
                                    fc = (fb % 2) * FB + j
                                    P.op("pe", lambda e, pt=pt, fc=fc, j=j, n=n, o0=o0, rows=rows, wo2=wo2: e.matmul(pt[:rows, :], hid[:, fc, o0:o0 + rows], wo2[:, j, n * 512:(n + 1) * 512],
                                                                                                                  start=(j == 0), stop=(j == FB - 1)),
                                         reads=[f"hid{fc}", wo2k], writes=[pk])
                                first = (ex == 0 and fb == 0)
                                if ffn == "moe":
                                    gcol = gates[:rows, ti, ex:ex + 1]
                                    if first:
                                        P.op("dve", lambda e, pt=pt, ti=ti, n=n, rows=rows, gcol=gcol: e.tensor_scalar(acc[:rows, ti, n * 512:(n + 1) * 512], pt[:rows, :], gcol, None, ALU.mult),
                                             reads=[pk, f"gates{ti}"], writes=[f"acc{ti}_{n}"])
                                    else:
                                        P.op("dve", lambda e, pt=pt, ti=ti, n=n, rows=rows, gcol=gcol: e.scalar_tensor_tensor(acc[:rows, ti, n * 512:(n + 1) * 512], pt[:rows, :], gcol,
                                                                                                                              acc[:rows, ti, n * 512:(n + 1) * 512], ALU.mult, ALU.add),
                                             reads=[pk, f"gates{ti}", f"acc{ti}_{n}"], writes=[f"acc{ti}_{n}"])
                                else:
                                    if first:
                                        P.op("dve", lambda e, pt=pt, ti=ti, n=n, rows=rows: e.tensor_copy(acc[:rows, ti, n * 512:(n + 1) * 512], pt[:rows, :]),
                                             reads=[pk], writes=[f"acc{ti}_{n}"])
                                    else:
                                        P.op("dve", lambda e, pt=pt, ti=ti, n=n, rows=rows: e.tensor_tensor(acc[:rows, ti, n * 512:(n + 1) * 512], pt[:rows, :], acc[:rows, ti, n * 512:(n + 1) * 512], ALU.add),
                                             reads=[pk, f"acc{ti}_{n}"], writes=[f"acc{ti}_{n}"])

                    if pending is not None:
                        pending()
                    pending = second_stage
            if pending is not None:
                pending()
            A_ = acc
            for ti, (r0, rows, ci) in enumerate(grp):
                gl, glk = mods[(ci, 5)]
                xot, xok = xo.next()
                P.op("dve", lambda e, xot=xot, ti=ti, rows=rows, gl=gl: e.tensor_tensor(xot[:rows], A_[:rows, ti, :], gl[:rows], ALU.mult),
                     reads=[f"acc{ti}_0", f"acc{ti}_1", glk], writes=[xok])
                P.op("pool", lambda e, xot=xot, ti=ti, rows=rows: e.tensor_tensor(xot[:rows], xot[:rows], x1[:rows, ti, :], ALU.add), reads=[xok, f"x1_{ti}"], writes=[xok])
                if last:
                    sq, sqk = nrm.sq.next()
                    ss, ssk = st1.next()
                    P.op("act", lambda e, sq=sq, xot=xot, ss=ss, rows=rows: e.activation(out=sq[:rows], in_=xot[:rows], func=AF.Square, accum_out=ss[:rows]), reads=[xok], writes=[sqk, ssk])
                    P.op("dve", lambda e, ss=ss, rows=rows: e.tensor_scalar(ss[:rows], ss[:rows], 1.0 / D, 1e-6, ALU.mult, ALU.add), reads=[ssk], writes=[ssk])
                    P.op("act", lambda e, ss=ss, rows=rows: e.activation(out=ss[:rows], in_=ss[:rows], func=AF.Sqrt), reads=[ssk], writes=[ssk])
                    P.op("dve", lambda e, ss=ss, rows=rows: e.reciprocal(ss[:rows], ss[:rows]), reads=[ssk], writes=[ssk])
                    P.op("dve", lambda e, xot=xot, ss=ss, rows=rows: e.scalar_tensor_tensor(xot[:rows], xot[:rows], ss[:rows, 0:1], fgb[:rows], ALU.mult, ALU.mult), reads=[xok, ssk, "fgb"], writes=[xok])
                evs.append(P.dma("act", out[r0:r0 + rows, :], xot[:rows, :], reads=[xok]))
        P.finish(evs)
        P.emit()
    return nc


def build_B_ml(T, GC=5):
    NJ = 4
    NCH = T // 128
    NSEG = NCH // GC
    assert NSEG * GC == NCH
    R = NJ * NSEG
    GW = GC * 128
    nc = bass.Bass("TRN2", target_bir_lowering=False)
    qT = dram_in(nc, "qT", [NJ, 64, T])
    kT = dram_in(nc, "kT", [NJ, 64, T])
    ktok = dram_in(nc, "ktok", [NJ, T, 64])
    vtok = dram_in(nc, "vtok", [NJ, T, 128])
    ipre = dram_in(nc, "ipre", [R, GW])
    fpre = dram_in(nc, "fpre", [R, GW])
    gbias = dram_in(nc, "gbias", [R, 2])
    hout = dram_out(nc, "h", [NJ, T, 128])
    s_be = nc.dram_tensor("s_be", [R, GC], F32).ap()
    s_ml = nc.dram_tensor("s_ml", [R, GC], F32).ap()
    s_mi = nc.dram_tensor("s_mi", [NJ, NCH], F32).ap()
    with contextlib.ExitStack() as st:
        P = Prog(nc, st, n_epochs=8)
        identf, ifk = make_ident(P, "identf", F32)
        maskU, muk = make_tri(P, "maskU", True)
        it = P.sb("g_it", [R, GW], F32)
        ft = P.sb("g_ft", [R, GW], F32)
        gb = P.sb("g_gb", [R, 2], F32)
        P.dma("sp", it[:], ipre, writes=["it"])
        P.dma("sp", ft[:], fpre, writes=["ft"])
        P.dma("sp", gb[:], gbias, writes=["gb"])
        P.op("dve", lambda e: e.tensor_scalar(it[:], it[:], gb[:, 0:1], None, ALU.add), reads=["it", "gb"], writes=["it"])
        P.op("dve", lambda e: e.tensor_scalar(ft[:], ft[:], gb[:, 1:2], None, ALU.add), reads=["ft", "gb"], writes=["ft"])
        P.op("act", lambda e: e.activation(out=ft[:], in_=ft[:], func=AF.Exp, scale=-1.0), reads=["ft"], writes=["ft"])
        P.op("act", lambda e: e.activation(out=ft[:], in_=ft[:], func=AF.Ln, bias=1.0), reads=["ft"], writes=["ft"])
        rmask = P.sb("g_rm", [R, GW], F32)
        nmask = P.sb("g_nm", [R, GW], F32)
        P.op("dve", lambda e: e.memset(rmask[:], 1.0), writes=["rmask"])
        P.op("dve", lambda e: e.memset(rmask[:, 0:GW:128], 0.0), reads=["rmask"], writes=["rmask"])
        P.op("dve", lambda e: e.memset(nmask[:], 0.0), writes=["nmask"])
        P.op("dve", lambda e: e.memset(nmask[:, 0:GW:128], -1e30), reads=["nmask"], writes=["nmask"])
        nb = P.sb("g_nb", [R, GW], F32)
        P.op("dve", lambda e: e.tensor_tensor_scan(nb[:], rmask[:], ft[:], 0.0, ALU.mult, ALU.add), reads=["rmask", "ft"], writes=["nb"])
        u = P.sb("g_u", [R, GW], F32)
        P.op("dve", lambda e: e.tensor_tensor(u[:], it[:], nb[:], ALU.add), reads=["it", "nb"], writes=["u"])
        cu = P.sb("g_cu", [R, GW], F32)
        P.op("dve", lambda e: e.tensor_tensor_scan(cu[:], nmask[:], u[:], 0.0, ALU.add, ALU.max), reads=["nmask", "u"], writes=["cu"])
        cmax = P.sb("g_cmax", [R, GC], F32)
        bend = P.sb("g_bend", [R, GC], F32)
        mloc = P.sb("g_mloc", [R, GC], F32)
        P.op("dve", lambda e: e.tensor_copy(cmax[:], cu[:, 127:GW:128]), reads=["cu"], writes=["cmax"])
        P.op("dve", lambda e: e.tensor_scalar(bend[:], nb[:, 127:GW:128], -1.0, None, ALU.mult), reads=["nb"], writes=["bend"])
        P.op("dve", lambda e: e.tensor_tensor(mloc[:], bend[:], cmax[:], ALU.add), reads=["bend", "cmax"], writes=["mloc"])
        E = P.sb("g_E", [R, GW], F32)
        P.op("dve", lambda e: e.tensor_tensor(E[:].rearrange("r (c t) -> r c t", t=128), u[:].rearrange("r (c t) -> r c t", t=128),
                                              cmax[:].unsqueeze(2).broadcast_to([R, GC, 128]), ALU.subtract), reads=["u", "cmax"], writes=["E"])
        P.op("act", lambda e: e.activation(out=E[:], in_=E[:], func=AF.Exp), reads=["E"], writes=["E"])
        e1 = P.dma("sp", s_be, bend[:], reads=["bend"], writes=["s_be"])
        e2 = P.dma("sp", s_ml, mloc[:], reads=["mloc"], writes=["s_ml"])
        be4 = P.sb("g_be4", [NJ, NCH], F32)
        ml4 = P.sb("g_ml4", [NJ, NCH], F32)
        P.dma("sp", be4[:], s_be.rearrange("(j s) c -> j (s c)", j=NJ), reads=["s_be"], writes=["be4"])
        P.dma("sp", ml4[:], s_ml.rearrange("(j s) c -> j (s c)", j=NJ), reads=["s_ml"], writes=["ml4"])
        mo4 = P.sb("g_mo4", [NJ, NCH], F32)
        mi4 = P.sb("g_mi4", [NJ, NCH], F32)
        P.op("dve", lambda e: e.tensor_tensor_scan(mo4[:], be4[:], ml4[:], 0.0, ALU.add, ALU.max), reads=["be4", "ml4"], writes=["mo4"])
        P.op("dve", lambda e: e.memset(mi4[:, 0:1], 0.0), writes=["mi4"])
        if NCH > 1:
            P.op("dve", lambda e: e.tensor_copy(mi4[:, 1:NCH], mo4[:, 0:NCH - 1]), reads=["mo4", "mi4"], writes=["mi4"])
        as4 = P.sb("g_as4", [NJ, 2, NCH], F32)
        P.op("dve", lambda e: e.tensor_tensor(as4[:, 0, :], be4[:], mi4[:], ALU.add), reads=["be4", "mi4"], writes=["as4"])
        P.op("dve", lambda e: e.tensor_tensor(as4[:, 0, :], as4[:, 0, :], mo4[:], ALU.subtract), reads=["as4", "mo4"], writes=["as4"])
        P.op("dve", lambda e: e.tensor_tensor(as4[:, 1, :], ml4[:], mo4[:], ALU.subtract), reads=["ml4", "mo4", "as4"], writes=["as4"])
        P.op("act", lambda e: e.activation(out=as4[:], in_=as4[:], func=AF.Exp), reads=["as4"], writes=["as4"])
        ASb = P.sb("g_ASb", [64, NJ, 2, NCH], F32)
        sel = P.sb("g_sel", [NJ, NJ, 64], F32)
        P.op("pool", lambda e: e.memset(sel[:], 0.0), writes=["sel"])
        for j in range(NJ):
            P.op("pool", lambda e, j=j: e.affine_select(out=sel[:, j, :], in_=sel[:, j, :], pattern=[[0, 64]], compare_op=ALU.not_equal,
                                                        fill=1.0, base=-j, channel_multiplier=1), reads=["sel"], writes=["sel"])
        psG = Rot(P, "psG", [128, 512], F32, 2, psum=True)
        for j in range(NJ):
            pt, pk = psG.next()
            P.op("pe", lambda e, pt=pt, j=j: e.matmul(pt[0:64, 0:2 * NCH], sel[:, j, :], as4[:].rearrange("j a c -> j (a c)"), start=True, stop=True),
                 reads=["sel", "as4"], writes=[pk])
            P.op("dve", lambda e, pt=pt, j=j: e.tensor_copy(ASb[:, j, :, :].rearrange("p a c -> p (a c)"), pt[0:64, 0:2 * NCH]), reads=[pk], writes=["ASb"])
        P.dma("sp", s_mi, mi4[:], reads=["mi4"], writes=["s_mi"])
        mi = P.sb("g_mi", [R, GC], F32)
        P.dma("sp", mi[:], s_mi.rearrange("j (s c) -> (j s) c", c=GC), reads=["s_mi"], writes=["mi"])
        M = P.sb("g_M", [R, GW], F32)
        Mv = M[:].rearrange("r (c t) -> r c t", t=128)
        P.op("dve", lambda e: e.tensor_tensor(Mv, cu[:].rearrange("r (c t) -> r c t", t=128), mi[:].unsqueeze(2).broadcast_to([R, GC, 128]), ALU.max),
             reads=["cu", "mi"], writes=["M"])
        P.op("dve", lambda e: e.tensor_tensor(cu[:].rearrange("r (c t) -> r c t", t=128), cmax[:].unsqueeze(2).broadcast_to([R, GC, 128]), Mv, ALU.subtract),
             reads=["cmax", "M", "cu"], writes=["cu"])
        P.op("act", lambda e: e.activation(out=cu[:], in_=cu[:], func=AF.Exp), reads=["cu"], writes=["cu"])
        P.op("dve", lambda e: e.tensor_tensor(u[:].rearrange("r (c t) -> r c t", t=128), mi[:].unsqueeze(2).broadcast_to([R, GC, 128]), Mv, ALU.subtract),
             reads=["mi", "M", "u"], writes=["u"])
        P.op("act", lambda e: e.activation(out=u[:], in_=u[:], func=AF.Exp), reads=["u"], writes=["u"])
        P.op("dve", lambda e: e.tensor_tensor(nb[:], nb[:], M[:], ALU.subtract), reads=["nb", "M"], writes=["nb"])
        P.op("act", lambda e: e.activation(out=nb[:], in_=nb[:], func=AF.Exp), reads=["nb"], writes=["nb"])
        TM = P.sb("g_TM", [128, 4, GC, R], F32)
        for qi, (src, sk) in enumerate(((E, "E"), (cu, "cu"), (u, "u"), (nb, "nb"))):
            for j in range(GC):
                pt, pk = psG.next()
                P.op("pe", lambda e, pt=pt, src=src, j=j: e.transpose(pt[:, 0:R], src[:, j * 128:(j + 1) * 128], identf[:R, :R]), reads=[sk, ifk], writes=[pk])
                P.op("dve", lambda e, pt=pt, qi=qi, j=j: e.tensor_copy(TM[:, qi, j, :], pt[:, 0:R]), reads=[pk], writes=["TM"])
        P.barrier()
        Cst = [P.sb(f"Cst{j}", [64, 129], F32) for j in range(NJ)]
        Cbf = [P.sb(f"Cbf{j}", [64, 129], BF16) for j in range(NJ)]
        for j in range(NJ):
            P.op("dve", lambda e, j=j: e.memset(Cst[j][:], 0.0), writes=[f"Cst{j}"])
            P.op("pool", lambda e, j=j: e.memset(Cbf[j][:], 0.0), writes=[f"Cbf{j}"])
        NB_ = 2
        qg = [Rot(P, f"qg{j}_", [64, GW], BF16, NB_) for j in range(NJ)]
        kg = [Rot(P, f"kg{j}_", [64, GW], BF16, NB_) for j in range(NJ)]
        ktg = [Rot(P, f"ktg{j}_", [128, GC, 64], BF16, NB_) for j in range(NJ)]
        vg = [Rot(P, f"vg{j}_", [128, GC, 129], BF16, NB_) for j in range(NJ)]
        hg = [Rot(P, f"hg{j}_", [128, GC, 128], F32, NB_) for j in range(NJ)]
        for j in range(NJ):
            for (t, k) in zip(vg[j].tiles, vg[j].keys):
                P.op("pool", lambda e, t=t: e.memset(t[:, :, 128:129], 1.0), writes=[k])
        psS = Rot(P, "psS", [128, 512], F32, 2, psum=True)
        psO = Rot(P, "psO", [128, 512], F32, 2, psum=True)
        psC = Rot(P, "psC", [128, 512], F32, 2, psum=True)
        Sm = Rot(P, "Sm", [128, 128], BF16, 3)
        ev = Rot(P, "ev", [128, 129], BF16, 3)
        n1 = Rot(P, "n1", [128, 129], F32, 3)
        dn = Rot(P, "dn", [128, 1], F32, 4)
        tC = Rot(P, "tC", [64, 129], F32, 2)
        evs = []
        for seg in range(NSEG):
            t0 = seg * GW
            cur = []
            for j in range(NJ):
                q_, qk = qg[j].next()
                k_, kk = kg[j].next()
                kt_, ktk = ktg[j].next()
                v_, vk = vg[j].next()
                h_, hk = hg[j].next()
                P.dma("pool", q_[:], qT[j, :, t0:t0 + GW], writes=[qk])
                P.dma("pool", k_[:], kT[j, :, t0:t0 + GW], writes=[kk])
                P.dma("pool", kt_[:], ktok[j, t0:t0 + GW, :].rearrange("(c p) d -> p c d", p=128), writes=[ktk])
                P.dma("pool", v_[:, :, 0:128], vtok[j, t0:t0 + GW, :].rearrange("(c p) d -> p c d", p=128), writes=[vk])
                cur.append((q_, qk, k_, kk, kt_, ktk, v_, vk, h_, hk))
            for c in range(GC):
                cg = seg * GC + c
                for j in range(NJ):
                    q_, qk, k_, kk, kt_, ktk, v_, vk, h_, hk = cur[j]
                    r = j * NSEG + seg
                    cs = slice(c * 128, (c + 1) * 128)
                    pS, pSk = psS.next()
                    P.op("pe", lambda e, pS=pS, k_=k_, q_=q_, cs=cs: e.matmul(pS[:, 0:128], k_[:, cs], q_[:, cs], start=True, stop=True), reads=[kk, qk], writes=[pSk])
                    sm, smk = Sm.next()
                    P.op("dve", lambda e, sm=sm, pS=pS: e.tensor_tensor(sm[:], pS[:, 0:128], maskU[:], ALU.mult), reads=[pSk, muk], writes=[smk])
                    e_, ek = ev.next()
                    P.op("act", lambda e, e_=e_, v_=v_, c=c, r=r: e.activation(out=e_[:], in_=v_[:, c, :], func=AF.Copy, scale=TM[:, 0, c, r:r + 1]), reads=[vk, "TM"], writes=[ek])
                    pO, pOk = psO.next()
                    P.op("pe", lambda e, pO=pO, sm=sm, e_=e_: e.matmul(pO[:, 0:129], sm[:], e_[:], start=True, stop=True), reads=[smk, ek], writes=[pOk])
                    P.op("pe", lambda e, pO=pO, q_=q_, cs=cs, j=j: e.matmul(pO[:, 256:385], q_[:, cs], Cbf[j][:], start=True, stop=True), reads=[qk, f"Cbf{j}"], writes=[pOk])
                    n_, nk = n1.next()
                    P.op("dve", lambda e, n_=n_, pO=pO, c=c, r=r: e.tensor_scalar(n_[:], pO[:, 0:129], TM[:, 1, c, r:r + 1], None, ALU.mult), reads=[pOk, "TM"], writes=[nk])
                    P.op("dve", lambda e, n_=n_, pO=pO, c=c, r=r: e.scalar_tensor_tensor(n_[:], pO[:, 256:385], TM[:, 2, c, r:r + 1], n_[:], ALU.mult, ALU.add),
                         reads=[pOk, "TM", nk], writes=[nk])
                    d_, dk = dn.next()
                    P.op("act", lambda e, d_=d_, n_=n_: e.activation(out=d_[:], in_=n_[:, 128:129], func=AF.Abs), reads=[nk], writes=[dk])
                    P.op("dve", lambda e, d_=d_, c=c, r=r: e.tensor_tensor(d_[:], d_[:], TM[:, 3, c, r:r + 1], ALU.max), reads=[dk, "TM"], writes=[dk])
                    P.op("dve", lambda e, d_=d_: e.reciprocal(d_[:], d_[:]), reads=[dk], writes=[dk])
                    P.op("act", lambda e, h_=h_, n_=n_, d_=d_, c=c: e.activation(out=h_[:, c, :], in_=n_[:, 0:128], func=AF.Copy, scale=d_[:, 0:1]), reads=[nk, dk], writes=[hk])
                    pC, pCk = psC.next()
                    P.op("pe", lambda e, pC=pC, kt_=kt_, e_=e_, c=c: e.matmul(pC[0:64, 0:129], kt_[:, c, :], e_[:], start=True, stop=True), reads=[ktk, ek], writes=[pCk])
                    t_, tk = tC.next()
                    P.op("dve", lambda e, t_=t_, pC=pC, j=j, cg=cg: e.tensor_scalar(t_[:], pC[0:64, 0:129], ASb[:, j, 1, cg:cg + 1], None, ALU.mult), reads=[pCk, "ASb"], writes=[tk])
                    P.op("dve", lambda e, t_=t_, j=j, cg=cg: e.scalar_tensor_tensor(Cst[j][:], Cst[j][:], ASb[:, j, 0, cg:cg + 1], t_[:], ALU.mult, ALU.add),
                         reads=[f"Cst{j}", "ASb", tk], writes=[f"Cst{j}"])
                    P.op("act", lambda e, j=j: e.copy(Cbf[j][:], Cst[j][:]), reads=[f"Cst{j}"], writes=[f"Cbf{j}"])
            for j in range(NJ):
                h_, hk = cur[j][8], cur[j][9]
                evs.append(P.dma("sp", hout[j, t0:t0 + GW, :].rearrange("(c p) d -> p c d", p=128), h_[:], reads=[hk]))
        P.finish(evs)
        P.emit()
    return nc


def build_B_df(T, NCX, lam_init):
    NP = 2
    NCH = T // 128
    nc = bass.Bass("TRN2", target_bir_lowering=False)
    qT = dram_in(nc, "qT", [NP, 64, 2, T])
    kT = dram_in(nc, "kT", [NP, 64, 2, T])
    vt = dram_in(nc, "v", [NP, T, 128])
    lam = dram_in(nc, "lam", [1, 256])
    out = dram_out(nc, "o", [NP, T, 128])
    with contextlib.ExitStack() as st:
        P = Prog(nc, st, n_epochs=8)
        lb = P.sb("lb", [128, 4, 64], F32)
        P.dma("sp", lb[:].rearrange("p a b -> p (a b)"), lam[0:1, :].partition_broadcast(128), writes=["lb"])
        lt = P.sb("lt", [128, 2, 64], F32)
        ls = P.sb("ls", [128, 2], F32)
        nlam = P.sb("nlam", [128, 1], F32)
        P.op("dve", lambda e: e.tensor_tensor(lt[:, 0, :], lb[:, 0, :], lb[:, 1, :], ALU.mult), reads=["lb"], writes=["lt"])
        P.op("dve", lambda e: e.tensor_tensor(lt[:, 1, :], lb[:, 2, :], lb[:, 3, :], ALU.mult), reads=["lb", "lt"], writes=["lt"])
        P.op("dve", lambda e: e.tensor_reduce(ls[:], lt[:], AX.X, ALU.add), reads=["lt"], writes=["ls"])
        P.op("act", lambda e: e.activation(out=ls[:], in_=ls[:], func=AF.Exp), reads=["ls"], writes=["ls"])
        P.op("dve", lambda e: e.tensor_tensor(nlam[:], ls[:, 1:2], ls[:, 0:1], ALU.subtract), reads=["ls"], writes=["nlam"])
        P.op("dve", lambda e: e.tensor_scalar(nlam[:], nlam[:], -lam_init, None, ALU.add), reads=["nlam"], writes=["nlam"])
        k12 = P.sb("k12", [64, 2, T], BF16)
        va = P.sb("va", [128, NCH, 129], BF16)
        P.op("pool", lambda e: e.memset(va[:, :, 128:129], 1.0), writes=["va"])
        q12 = Rot(P, "q12_", [64, 2, 512], BF16, 2)
        pex = Rot(P, "pex", [128, 512], BF16, 6)
        psS = Rot(P, "psS", [128, 512], F32, 4, psum=True)
        psO = [P.ps(f"psO{i}", [128, 512], F32) for i in range(4)]
        ot = Rot(P, "ot", [128, 4, 128], F32, 2)
        t1 = Rot(P, "t1_", [128, 128], F32, 2)
        rd = Rot(P, "rd", [128, 2], F32, 4)
        evs = []
        qtiles = [(0, NCX, NCX // 128)]
        q0 = NCX
        while q0 < T:
            qw = min(512, T - q0)
            qtiles.append((q0, qw, NCH))
            q0 += qw
        CW = 2048
        for p in range(NP):
            for c0 in range(0, T, CW):
                cw = min(CW, T - c0)
                P.dma("pool", k12[:, :, c0:c0 + cw], kT[p, :, :, c0:c0 + cw], writes=["k12"])
                P.dma("pool", va[:, c0 // 128:(c0 + cw) // 128, 0:128], vt[p, c0:c0 + cw, :].rearrange("(c q) d -> q c d", q=128), writes=["va"])
            for (q0, qw, nk) in qtiles:
                qq, qk = q12.next()
                P.dma("pool", qq[:, :, 0:qw], qT[p, :, :, q0:q0 + qw], writes=[qk])
                nq = qw // 128
                def emit_qk(kc, qq=qq, qk=qk, qw=qw):
                    pes = []
                    for sub in range(2):
                        pS, pSk = psS.next()
                        P.op("pe", lambda e, pS=pS, sub=sub, kc=kc, qq=qq, qw=qw: e.matmul(pS[:, 0:qw], k12[:, sub, kc * 128:(kc + 1) * 128],
                                                                                         qq[:, sub, 0:qw], start=True, stop=True),
                             reads=["k12", qk], writes=[pSk])
                        pe_, pek = pex.next()
                        P.op("act", lambda e, pe_=pe_, pS=pS, qw=qw: e.activation(out=pe_[:, 0:qw], in_=pS[:, 0:qw], func=AF.Exp, scale=0.125), reads=[pSk], writes=[pek])
                        pes.append((pe_, pek))
                    return pes
                nxt = emit_qk(0)
                for kc in range(nk):
                    pes = nxt
                    if kc + 1 < nk:
                        nxt = emit_qk(kc + 1)
                    for qi in range(nq):
                        for sub in range(2):
                            pe_, pek = pes[sub]
                            P.op("pe", lambda e, qi=qi, sub=sub, pe_=pe_, kc=kc, nk=nk: e.matmul(psO[qi][:, sub * 256:sub * 256 + 129], pe_[:, qi * 128:(qi + 1) * 128], va[:, kc, :],
                                                                                               start=(kc == 0 and sub == 0), stop=(kc == nk - 1), skip_group_check=True),
                                 reads=[pek, "va"], writes=[f"psO{qi}"])
                o_, ok_ = ot.next()
                for qi in range(nq):
                    r_, rk = rd.next()
                    P.op("dve", lambda e, r_=r_, qi=qi: e.reciprocal(r_[:, 0:1], psO[qi][:, 128:129]), reads=[f"psO{qi}"], writes=[rk])
                    P.op("dve", lambda e, r_=r_, qi=qi: e.reciprocal(r_[:, 1:2], psO[qi][:, 256 + 128:256 + 129]), reads=[f"psO{qi}", rk], writes=[rk])
                    P.op("dve", lambda e, r_=r_: e.tensor_tensor(r_[:, 1:2], r_[:, 1:2], nlam[:], ALU.mult), reads=[rk, "nlam"], writes=[rk])
                    t_, tk = t1.next()
                    P.op("dve", lambda e, t_=t_, r_=r_, qi=qi: e.tensor_scalar(t_[:], psO[qi][:, 0:128], r_[:, 0:1], None, ALU.mult), reads=[f"psO{qi}", rk], writes=[tk])
                    P.op("dve", lambda e, t_=t_, r_=r_, qi=qi, o_=o_: e.scalar_tensor_tensor(o_[:, qi, :], psO[qi][:, 256:384], r_[:, 1:2], t_[:], ALU.mult, ALU.add),
                         reads=[f"psO{qi}", rk, tk], writes=[ok_])
                evs.append(P.dma("sp", out[p, q0:q0 + qw, :].rearrange("(c q) d -> q c d", q=128), o_[:, 0:nq, :], reads=[ok_]))
        P.finish(evs)
        P.emit()
    return nc


def build_B_sw(T, NCX):
    NBLK = T // 128
    NCB = NCX // 128
    NLB = NBLK - NCB
    nc = bass.Bass("TRN2", target_bir_lowering=False)
    qT = dram_in(nc, "qT", [64, NBLK, 512])
    kT = dram_in(nc, "kT", [64, T])
    vt = dram_in(nc, "v", [T, 64])
    sinks = dram_in(nc, "sinks", [1, 4])
    out = dram_out(nc, "o", [T, 256])
    with contextlib.ExitStack() as st:
        P = Prog(nc, st, n_epochs=6)
        maskU, muk = make_tri(P, "maskU", True, BF16)
        maskL, mlk = make_tri(P, "maskL", False, BF16)
        es = P.sb("es", [128, 4], F32)
        P.dma("sp", es[:], sinks[0:1, :].partition_broadcast(128), writes=["es"])
        P.op("act", lambda e: e.activation(out=es[:], in_=es[:], func=AF.Exp), reads=["es"], writes=["es"])
        kk = P.sb("kk", [64, T], BF16)
        va = P.sb("va", [128, NBLK, 65], BF16)
        P.op("pool", lambda e: e.memset(va[:, :, 64:65], 1.0), writes=["va"])
        CW = 2048
        for c0 in range(0, T, CW):
            cw = min(CW, T - c0)
            P.dma("pool", kk[:, c0:c0 + cw], kT[:, c0:c0 + cw], writes=["kk"])
            P.dma("pool", va[:, c0 // 128:(c0 + cw) // 128, 0:64], vt[c0:c0 + cw, :].rearrange("(c q) d -> q c d", q=128), writes=["va"])
        qb = Rot(P, "qb", [64, 512], BF16, 3)
        psS = Rot(P, "psS", [128, 512], F32, 6, psum=True)
        psO = Rot(P, "psO", [128, 512], F32, 2, psum=True)
        pex = Rot(P, "pex", [128, 512], BF16, 10)
        dn = Rot(P, "dn", [128, 4], F32, 3)
        ot = Rot(P, "ot", [128, 4, 64], F32, 3)
        evs = []
        for n in range(NBLK):
            if n < NCB:
                chunks = [(c, None) for c in range(NCB)]
            else:
                m = n - NCB
                chunks = [(c, None) for c in range(NCB)]
                if m > 0:
                    chunks.append((n - 1, "L"))
                chunks.append((n, None))
                if m < NLB - 1:
                    chunks.append((n + 1, "U"))
            q_, qk = qb.next()
            P.dma("pool", q_[:], qT[:, n, :], writes=[qk])
            pes = []
            for (c, mk) in chunks:
                pS, pSk = psS.next()
                P.op("pe", lambda e, pS=pS, c=c, q_=q_: e.matmul(pS[:, :], kk[:, c * 128:(c + 1) * 128], q_[:], start=True, stop=True), reads=["kk", qk], writes=[pSk])
                pe_, pek = pex.next()
                P.op("act", lambda e, pe_=pe_, pS=pS: e.activation(out=pe_[:], in_=pS[:, :], func=AF.Exp, scale=0.125), reads=[pSk], writes=[pek])
                if mk is not None:
                    mt, mtk = (maskL, mlk) if mk == "L" else (maskU, muk)
                    P.op("dve", lambda e, pe_=pe_, mt=mt: e.tensor_tensor(pe_[:].rearrange("k (h q) -> k h q", q=128), pe_[:].rearrange("k (h q) -> k h q", q=128),
                                                                        mt[:].unsqueeze(1).broadcast_to([128, 4, 128]), ALU.mult), reads=[pek, mtk], writes=[pek])
                pes.append((pe_, pek, c))
            pO, pOk = psO.next()
            for h in range(4):
                for i_, (pe_, pek, c) in enumerate(pes):
                    P.op("pe", lambda e, pO=pO, h=h, pe_=pe_, c=c, i_=i_, nl=len(pes): e.matmul(pO[:, h * 65:(h + 1) * 65], pe_[:, h * 128:(h + 1) * 128], va[:, c, :],
                                                                                             start=(i_ == 0), stop=(i_ == nl - 1)),
                         reads=[pek, "va"], writes=[pOk])
            d_, dk = dn.next()
            pv = pO[:, 0:260].rearrange("q (h d) -> q h d", d=65)
            P.op("dve", lambda e, d_=d_, pv=pv: e.tensor_tensor(d_[:], pv[:, :, 64], es[:], ALU.add), reads=[pOk, "es"], writes=[dk])
            P.op("dve", lambda e, d_=d_: e.reciprocal(d_[:], d_[:]), reads=[dk], writes=[dk])
            o_, ok_ = ot.next()
            P.op("dve", lambda e, o_=o_, pv=pv, d_=d_: e.tensor_tensor(o_[:], pv[:, :, 0:64], d_[:].unsqueeze(2).broadcast_to([128, 4, 64]), ALU.mult), reads=[pOk, dk], writes=[ok_])
            evs.append(P.dma("sp", out[n * 128:(n + 1) * 128, :], o_[:].rearrange("q h d -> q (h d)"), reads=[ok_]))
        P.finish(evs)
        P.emit()
    return nc


def run_diff(seq, df_lam, B, T, NCX, lam_init):
    ncB = _get(("Bdf", T, NCX, lam_init), lambda: build_B_df(T, NCX, lam_init))
    lamv = _f32(np.asarray(df_lam[0]).reshape(1, 256))
    maps = []
    for cc in range(NCORE):
        qT = np.empty((2, 64, 2, T), np.float32)
        kT = np.empty((2, 64, 2, T), np.float32)
        v = np.empty((2, T, 128), np.float32)
        for lp in range(2):
            pi = cc * 2 + lp
            b, h = pi // 8, pi % 8
            qT[lp] = np.transpose(seq[b][:, h * 128:(h + 1) * 128].reshape(T, 2, 64), (2, 1, 0))
            kT[lp] = np.transpose(seq[b][:, 1024 + h * 128:1024 + (h + 1) * 128].reshape(T, 2, 64), (2, 1, 0))
            v[lp] = seq[b][:, 2048 + h * 128:2048 + (h + 1) * 128]
        maps.append({"qT": qT, "kT": kT, "v": v, "lam": lamv})
    res = _run(ncB, maps)
    o_all = np.empty((B, T, D), np.float32)
    for cc in range(NCORE):
        for lp in range(2):
            pi = cc * 2 + lp
            b, h = pi // 8, pi % 8
            o_all[b, :, h * 128:(h + 1) * 128] = res[cc]["o"][lp]
    return o_all


def run_swa(seq, sw_sinks, B, T, NCX):
    ncB = _get(("Bsw", T, NCX), lambda: build_B_sw(T, NCX))
    NBLK = T // 128
    maps = []
    for cc in range(NCORE):
        b, kv = cc // 4, cc % 4
        q = seq[b][:, kv * 256:(kv + 1) * 256].reshape(NBLK, 128, 4, 64)
        qT = _f32(np.transpose(q, (3, 0, 2, 1)).reshape(64, NBLK, 512))
        kT = _f32(seq[b][:, 1024 + kv * 64:1024 + (kv + 1) * 64].T)
        v = _f32(seq[b][:, 1280 + kv * 64:1280 + (kv + 1) * 64])
        maps.append({"qT": qT, "kT": kT, "v": v, "sinks": _f32(np.asarray(sw_sinks[0])[None, kv * 4:(kv + 1) * 4])})
    res = _run(ncB, maps)
    o_all = np.empty((B, T, D), np.float32)
    for cc in range(NCORE):
        b, kv = cc // 4, cc % 4
        o_all[b, :, kv * 256:(kv + 1) * 256] = res[cc]["o"]
    return o_all


_NC_CACHE = {}
_DBG = {}


def _get(key, fn):
    if key not in _NC_CACHE:
        _NC_CACHE[key] = fn()
    return _NC_CACHE[key]


def _run(nc, in_maps):
    res = run_bass_kernel_spmd(nc, in_maps, core_ids=list(range(NCORE)))
    return res.results


def _f32(a):
    return np.ascontiguousarray(a, dtype=np.float32)


def kernel(x, c, ctx, c_ctx, ada_w, ada_b, norm1, norm2, ml_w_in, ml_gate_b, ml_hnorm, ml_w_out,
           df_w_in, df_lam, df_hnorm, df_w_out, sw_w_in, sw_sinks, sw_w_out,
           ffn_w_in, ffn_w_out, moe_router, moe_w_in, moe_w_out, final_norm, depth=4, return_state=False):
    x = np.asarray(x, np.float32)
    xc = np.asarray(ctx, np.float32)
    B, L, _ = x.shape
    NCX = xc.shape[1]
    Ls = L // 4
    Cs = NCX // 4
    T = NCX + L
    pos_all = np.arange(L, dtype=np.float32)
    conds = [_f32(np.stack([np.asarray(c)[cc // 4], np.asarray(c_ctx)])) for cc in range(NCORE)]

    def shard_tok(lat, cx):
        outs = []
        for cc in range(NCORE):
            b, s = cc // 4, cc % 4
            outs.append(_f32(np.concatenate([lat[b, s * Ls:(s + 1) * Ls], cx[b, s * Cs:(s + 1) * Cs]], axis=0)))
        return outs

    def unshard_tok(outs, width):
        lat = np.empty((B, L, width), np.float32)
        cx = np.empty((B, NCX, width), np.float32)
        for cc in range(NCORE):
            b, s = cc // 4, cc % 4
            lat[b, s * Ls:(s + 1) * Ls] = outs[cc][:Ls]
            cx[b, s * Cs:(s + 1) * Cs] = outs[cc][Ls:]
        return lat, cx

    for i in range(depth):
        last = i == 3
        kind = ("ml", "df", "sw")[i % 3]
        jm = i // 3
        w_in = {"ml": ml_w_in, "df": df_w_in, "sw": sw_w_in}[kind][jm if kind == "ml" else 0]
        w_in = _f32(w_in)
        N = w_in.shape[1]
        xs = shard_tok(x, xc)
        aw = _f32(ada_w[i])
        ab = _f32(np.asarray(ada_b[i])[None, :])
        ncA = _get(("A", Ls, Cs, N, kind), lambda: build_A(Ls, Cs, N, kind))
        maps = []
        for cc in range(NCORE):
            m = {"x": xs[cc], "cond": conds[cc], "ada_w": aw, "ada_b": ab, "norm_g": _f32(np.asarray(norm1[i])[None, :]), "w_in": w_in}
            if kind in ("df", "sw"):
                s = cc % 4
                m["pos"] = _f32(pos_all[s * Ls:(s + 1) * Ls])
            maps.append(m)
        res = _run(ncA, maps)
        p_lat, p_ctx = unshard_tok([r["proj"] for r in res], N)
        seq = np.concatenate([p_ctx, p_lat], axis=1)
        cmaps = []
        if kind == "ml":
            GC = 5
            NCH = T // 128
            NSEG = NCH // GC
            seq_b = np.concatenate([p_ctx[:, ::-1], p_lat[:, ::-1]], axis=1)
            gbv = np.asarray(ml_gate_b[jm], np.float32)
            maps = []
            for cc in range(NCORE):
                qT = np.empty((4, 64, T), np.float32)
                kT = np.empty((4, 64, T), np.float32)
                kt = np.empty((4, T, 64), np.float32)
                vt = np.empty((4, T, 128), np.float32)
                ip = np.empty((4 * NSEG, GC * 128), np.float32)
                fp = np.empty((4 * NSEG, GC * 128), np.float32)
                gbs = np.empty((4 * NSEG, 2), np.float32)
                for lp in range(2):
                    pi = cc * 2 + lp
                    b, h = pi // 8, pi % 8
                    for d in range(2):
                        j = lp * 2 + d
                        sq = (seq, seq_b)[d][b]
                        qT[j] = sq[:, h * 64:(h + 1) * 64].T
                        kT[j] = sq[:, 512 + h * 64:512 + (h + 1) * 64].T
                        kt[j] = sq[:, 512 + h * 64:512 + (h + 1) * 64]
                        vt[j] = sq[:, 1024 + h * 128:1024 + (h + 1) * 128]
                        ip[j * NSEG:(j + 1) * NSEG] = sq[:, 3072 + (2 * d) * 8 + h].reshape(NSEG, GC * 128)
                        fp[j * NSEG:(j + 1) * NSEG] = sq[:, 3072 + (2 * d + 1) * 8 + h].reshape(NSEG, GC * 128)
                        gbs[j * NSEG:(j + 1) * NSEG, 0] = gbv[2 * d, h]
                        gbs[j * NSEG:(j + 1) * NSEG, 1] = gbv[2 * d + 1, h]
                maps.append({"qT": qT, "kT": kT, "ktok": kt, "vtok": vt, "ipre": ip, "fpre": fp, "gbias": gbs})
            ncB = _get(("Bml", T), lambda: build_B_ml(T, GC))
            res = _run(ncB, maps)
            hf = np.empty((B, T, D), np.float32)
            hb = np.empty((B, T, D), np.float32)
            for cc in range(NCORE):
                hh = res[cc]["h"]
                for lp in range(2):
                    pi = cc * 2 + lp
                    b, h = pi // 8, pi % 8
                    hf[b, :, h * 128:(h + 1) * 128] = hh[lp * 2]
                    hb[b, :NCX, h * 128:(h + 1) * 128] = hh[lp * 2 + 1][:NCX][::-1]
                    hb[b, NCX:, h * 128:(h + 1) * 128] = hh[lp * 2 + 1][NCX:][::-1]
            _DBG.update(hf=hf, hb=hb, seq=seq)
            m1s = shard_tok(hf[:, NCX:], hf[:, :NCX])
            m2s = shard_tok(hb[:, NCX:], hb[:, :NCX])
            ogs = shard_tok(p_lat[:, :, 2048:3072], p_ctx[:, :, 2048:3072])
            extra = [{"m1": m1s[cc], "m2": m2s[cc], "og": ogs[cc], "hn": _f32(np.asarray(ml_hnorm[jm])[None, :])} for cc in range(NCORE)]
            w_out = _f32(ml_w_out[jm])
            lam_init = 0.0
        elif kind == "df":
            o_all = run_diff(seq, df_lam, B, T, NCX, 0.8 - 0.6 * math.exp(-0.3 * i))
            m1s = shard_tok(o_all[:, NCX:], o_all[:, :NCX])
            extra = [{"m1": m1s[cc], "hn": _f32(np.asarray(df_hnorm[0])[None, :])} for cc in range(NCORE)]
            w_out = _f32(df_w_out[0])
            lam_init = 0.8 - 0.6 * math.exp(-0.3 * i)
        else:
            o_all = run_swa(seq, sw_sinks, B, T, NCX)
            m1s = shard_tok(o_all[:, NCX:], o_all[:, :NCX])
            extra = [{"m1": m1s[cc]} for cc in range(NCORE)]
            w_out = _f32(sw_w_out[0])
            lam_init = 0.0
        ffn = "dense" if i % 2 == 0 else "moe"
        jf = i // 2
        ncC = _get(("C", Ls, Cs, kind, ffn, last, lam_init), lambda: build_C(Ls, Cs, kind, ffn, last, lam_init))
        maps = []
        for cc in range(NCORE):
            m = {"x": xs[cc], "cond": conds[cc], "ada_w": aw, "ada_b": ab, "norm_g": _f32(np.asarray(norm2[i])[None, :]), "w_out": w_out}
            m.update(extra[cc])
            if ffn == "dense":
                m["f_in"] = _f32(np.asarray(ffn_w_in[jf])[None])
                m["f_out"] = _f32(np.asarray(ffn_w_out[jf])[None])
            else:
                m["f_in"] = _f32(moe_w_in[jf])
                m["f_out"] = _f32(moe_w_out[jf])
                m["router"] = _f32(moe_router[jf])
            if last:
                m["final_g"] = _f32(np.asarray(final_norm)[None, :])
            maps.append(m)
        res = _run(ncC, maps)
        x, xc_new = unshard_tok([r["xo"] for r in res], D)
        if not last:
            xc = xc_new
    if return_state:
        return x, xc
    return x
```
